# Optimizing a Trainium2 kernel written in Bass

```python
import jax, jax.numpy as jnp
from jax import lax
import numpy as np

D_MODEL = 2048
BATCH = 1
SEQ = 8192
DEPTH = 1
DEC_BATCH = 2
DEC_SEQ = 4096
PAST_LEN = 128

N_MEM = 256
CHUNK = 128
D_MIX = D_MODEL
D_GATE = D_MIX // 2
D_FOUR = D_MIX - D_GATE
HEAD_DIM = 128
N_GATE_HEADS = D_GATE // HEAD_DIM
N_FOUR_GROUPS = D_FOUR // HEAD_DIM
D_IN = 2 * D_GATE + D_FOUR
N_XHEADS = 4
XHEAD_DIM = D_MODEL // N_XHEADS
N_EXPERTS = 16
CAPACITY_FACTOR = 2
D_EXPERT = 2 * D_MODEL
EPS = 1e-6
LN_EPS = 1e-5

kernel_name = 'hymba_gmlp_fnet_ec_encoder'


def rmsnorm(x, g):
    xf = x.astype(jnp.float32)
    y = xf * lax.rsqrt(jnp.mean(xf * xf, axis=-1, keepdims=True) + EPS)
    return (y * g.astype(jnp.float32)).astype(x.dtype)


def layernorm(x, g, b):
    xf = x.astype(jnp.float32)
    mu = jnp.mean(xf, axis=-1, keepdims=True)
    xc = xf - mu
    var = jnp.mean(xc * xc, axis=-1, keepdims=True)
    y = xc * lax.rsqrt(var + LN_EPS) * g.astype(jnp.float32) + b.astype(jnp.float32)
    return y.astype(x.dtype)


def spatial_gating(p, w_s, b_s, ln_g, ln_b):
    B, S, _ = p.shape
    u, v = jnp.split(p, 2, axis=-1)
    u = u.reshape(B, S // CHUNK, CHUNK, N_GATE_HEADS, HEAD_DIM)
    v = layernorm(v.reshape(B, S // CHUNK, CHUNK, N_GATE_HEADS, HEAD_DIM), ln_g, ln_b)
    gate = jnp.einsum('hpq,bnqhd->bnphd', w_s, v) + jnp.swapaxes(b_s, 0, 1)[None, None, :, :, None]
    return (u * gate).reshape(B, S, D_GATE)


def fourier_mix(p, w_f):
    B, S, _ = p.shape
    z = p.astype(jnp.float32).reshape(B, S, N_FOUR_GROUPS, HEAD_DIM)
    f = jnp.fft.fft2(z, axes=(1, 3), norm='ortho').real.astype(p.dtype)
    return jnp.einsum('bsgc,gce->bsge', f, w_f).reshape(B, S, D_FOUR)


def token_mixer(xn, w_in, w_s, b_s, ln_g, ln_b, w_f, g_out_a, g_out_b, w_out):
    p = xn @ w_in
    pa = jax.nn.gelu(p[..., :2 * D_GATE])
    pb = p[..., 2 * D_GATE:]
    ya = rmsnorm(spatial_gating(pa, w_s, b_s, ln_g, ln_b), g_out_a)
    yb = rmsnorm(fourier_mix(pb, w_f), g_out_b)
    return jnp.concatenate([ya, yb], axis=-1) @ w_out


def memory_attention(xn, mem_n, w_q, w_kv, w_o):
    B, S, _ = xn.shape
    M = mem_n.shape[1]
    q = (xn @ w_q).reshape(B, S, N_XHEADS, XHEAD_DIM)
    k, v = jnp.split(mem_n @ w_kv, 2, axis=-1)
    k = k.reshape(B, M, N_XHEADS, XHEAD_DIM)
    v = v.reshape(B, M, N_XHEADS, XHEAD_DIM)
    s = jnp.einsum('bshd,bmhd->bhsm', q, k, preferred_element_type=jnp.float32) * (XHEAD_DIM ** -0.5)
    pr = jax.nn.softmax(s, axis=-1).astype(v.dtype)
    o = jnp.einsum('bhsm,bmhd->bshd', pr, v).reshape(B, S, D_MODEL)
    return o @ w_o


def expert_choice_ffn(xn, w_router, b_router, w_gate, w_up, w_down):
    B, S, D = xn.shape
    N = B * S
    cap = CAPACITY_FACTOR * N // N_EXPERTS
    t = xn.reshape(N, D)
    logits = jnp.einsum('nd,de->ne', t, w_router, preferred_element_type=jnp.float32) + b_router.astype(jnp.float32)
    aff = jax.nn.softmax(logits, axis=-1)
    gate, idx = lax.top_k(aff.T, cap)
    xs = t[idx]
    h = jax.nn.silu(jnp.einsum('ecd,edf->ecf', xs, w_gate)) * jnp.einsum('ecd,edf->ecf', xs, w_up)
    ye = jnp.einsum('ecf,efd->ecd', h, w_down) * gate[..., None].astype(t.dtype)
    out = jnp.zeros_like(t).at[idx.reshape(-1)].add(ye.reshape(-1, D))
    return out.reshape(B, S, D)


def encoder_trunk(x, mem, norm_mix, w_in, w_spatial, b_spatial, ln_v_g, ln_v_b, w_fourier,
                  norm_out_a, norm_out_b, w_out, norm_attn, norm_mem, w_q, w_kv, w_o,
                  norm_ffn, w_router, b_router, w_gate, w_up, w_down, norm_final):
    h = x
    for l in range(DEPTH):
        h = h + token_mixer(rmsnorm(h, norm_mix[l]), w_in[l], w_spatial[l], b_spatial[l], ln_v_g[l], ln_v_b[l],
                            w_fourier[l], norm_out_a[l], norm_out_b[l], w_out[l])
        h = h + memory_attention(rmsnorm(h, norm_attn[l]), rmsnorm(mem, norm_mem[l]), w_q[l], w_kv[l], w_o[l])
        h = h + expert_choice_ffn(rmsnorm(h, norm_ffn[l]), w_router[l], b_router[l], w_gate[l], w_up[l], w_down[l])
    return rmsnorm(h, norm_final)


def setup_inputs(seed: int = 0) -> dict:
    key = jax.random.key(seed)
    ks = jax.random.split(key, 26)
    f32 = jnp.float32
    L, D = DEPTH, D_MODEL

    def nrm(k, shape, scale):
        return jax.random.normal(k, shape, f32) * scale

    def gain(k, shape):
        return 1.0 + 0.02 * jax.random.normal(k, shape, f32)

    return {
        'x_prompt': nrm(ks[0], (BATCH, SEQ, D), 1.0),
        'x_sample': nrm(ks[1], (DEC_BATCH, DEC_SEQ, D), 1.0),
        'mem_prompt': nrm(ks[2], (BATCH, N_MEM, D), 1.0),
        'mem_sample': nrm(ks[3], (DEC_BATCH, N_MEM, D), 1.0),
        'norm_mix': gain(ks[4], (L, D)),
        'w_in': nrm(ks[5], (L, D, D_IN), D ** -0.5),
        'w_spatial': nrm(ks[6], (L, N_GATE_HEADS, CHUNK, CHUNK), CHUNK ** -0.5),
        'b_spatial': gain(ks[7], (L, N_GATE_HEADS, CHUNK)),
        'ln_v_g': gain(ks[8], (L, N_GATE_HEADS, HEAD_DIM)),
        'ln_v_b': nrm(ks[9], (L, N_GATE_HEADS, HEAD_DIM), 0.02),
        'w_fourier': nrm(ks[10], (L, N_FOUR_GROUPS, HEAD_DIM, HEAD_DIM), HEAD_DIM ** -0.5),
        'norm_out_a': gain(ks[11], (L, D_GATE)),
        'norm_out_b': gain(ks[12], (L, D_FOUR)),
        'w_out': nrm(ks[13], (L, D_MIX, D), D_MIX ** -0.5),
        'norm_attn': gain(ks[14], (L, D)),
        'norm_mem': gain(ks[15], (L, D)),
        'w_q': nrm(ks[16], (L, D, D), D ** -0.5),
        'w_kv': nrm(ks[17], (L, D, 2 * D), D ** -0.5),
        'w_o': nrm(ks[18], (L, D, D), D ** -0.5),
        'norm_ffn': gain(ks[19], (L, D)),
        'w_router': nrm(ks[20], (L, D, N_EXPERTS), D ** -0.5),
        'b_router': nrm(ks[21], (L, N_EXPERTS), 0.01),
        'w_gate': nrm(ks[22], (L, N_EXPERTS, D, D_EXPERT), D ** -0.5),
        'w_up': nrm(ks[23], (L, N_EXPERTS, D, D_EXPERT), D ** -0.5),
        'w_down': nrm(ks[24], (L, N_EXPERTS, D_EXPERT, D), D_EXPERT ** -0.5),
        'norm_final': gain(ks[25], (D,)),
    }


def reference(x_prompt, x_sample, mem_prompt, mem_sample, norm_mix, w_in, w_spatial, b_spatial, ln_v_g, ln_v_b,
              w_fourier, norm_out_a, norm_out_b, w_out, norm_attn, norm_mem, w_q, w_kv, w_o, norm_ffn,
              w_router, b_router, w_gate, w_up, w_down, norm_final):
    y_prompt = encoder_trunk(x_prompt, mem_prompt, norm_mix, w_in, w_spatial, b_spatial, ln_v_g, ln_v_b, w_fourier,
                             norm_out_a, norm_out_b, w_out, norm_attn, norm_mem, w_q, w_kv, w_o, norm_ffn,
                             w_router, b_router, w_gate, w_up, w_down, norm_final)
    y_sample = encoder_trunk(x_sample, mem_sample, norm_mix, w_in, w_spatial, b_spatial, ln_v_g, ln_v_b, w_fourier,
                             norm_out_a, norm_out_b, w_out, norm_attn, norm_mem, w_q, w_kv, w_o, norm_ffn,
                             w_router, b_router, w_gate, w_up, w_down, norm_final)
    return (y_prompt, y_sample)
```

```python
import contextlib
import math
import numpy as np
import concourse.bass as bass
import concourse.mybir as mybir
from concourse.bass_utils import run_bass_kernel_spmd

F32 = mybir.dt.float32
BF16 = mybir.dt.bfloat16
ALU = mybir.AluOpType
AF = mybir.ActivationFunctionType
AX = mybir.AxisListType

D = 2048
KD = 16
DIN = 3072
DG = 1024
NH = 8
NM = 256
DE = 4096
FC = 32
EPS = 1e-6
LN_EPS = 1e-5


class Bld:
    NPOOL = 24

    def __init__(self, nc):
        self.nc = nc
        self.st = contextlib.ExitStack()
        self.esem = {e: self.st.enter_context(nc.semaphore("s_" + e)) for e in ("pe", "act", "dve", "pool")}
        self.dsem = {q: [self.st.enter_context(nc.semaphore("d_%s_%d" % (q, k))) for k in range(self.NPOOL)]
                     for q in ("sp", "pool", "act")}
        self.cnt = {e: 0 for e in self.esem}
        self.dcnt = {q: 0 for q in self.dsem}
        self.duse = {q: [0] * self.NPOOL for q in self.dsem}
        self.waited = {e: {} for e in ("pe", "act", "dve", "pool", "sp")}
        self.reset()
        self.nseg = 0

    def reset(self):
        self.ops = []
        self.lastw = {}
        self.readers = {}

    def op(self, eng, fn, r=(), w=(), dma=False):
        i = len(self.ops)
        deps = set()
        for k in r:
            if k in self.lastw:
                deps.add(self.lastw[k])
        for k in w:
            if k in self.lastw:
                deps.add(self.lastw[k])
            deps.update(self.readers.get(k, ()))
        for k in r:
            self.readers.setdefault(k, []).append(i)
        for k in w:
            self.lastw[k] = i
            self.readers[k] = []
        self.ops.append(dict(eng=eng, fn=fn, deps=deps, dma=dma))
        return i

    def dma(self, q, out, in_, r=(), w=(), **kw):
        return self.op(q, lambda e: e.dma_start(out=out, in_=in_, **kw), r, w, dma=True)

    def flush(self, last=False):
        nc = self.nc
        ops = self.ops
        needed = [False] * len(ops)
        for o in ops:
            for d in o["deps"]:
                needed[d] = True
        barrier = [(("e", e), self.esem[e], self.cnt[e]) for e in self.esem]
        barrier += [(("d", q, k), self.dsem[q][k], 16 * self.duse[q][k])
                    for q in self.dsem for k in range(self.NPOOL)]
        last_on = {}
        for i, o in enumerate(ops):
            last_on[o["eng"]] = i
        for e, i in last_on.items():
            needed[i] = True
        for i, o in enumerate(ops):
            if o["dma"]:
                q = o["eng"]
                k = self.dcnt[q] % self.NPOOL
                self.dcnt[q] += 1
                o["prev"] = (self.dsem[q][k], 16 * self.duse[q][k])
                self.duse[q][k] += 1
                o["done"] = (self.dsem[q][k], 16 * self.duse[q][k])
                o["key"] = ("d", q, k)
            elif needed[i]:
                self.cnt[o["eng"]] += 1
                o["done"] = (self.esem[o["eng"]], self.cnt[o["eng"]])
                o["key"] = ("e", o["eng"])
            else:
                o["done"] = None
        final = [(("d", q, k), self.dsem[q][k], 16 * self.duse[q][k])
                 for q in self.dsem for k in range(self.NPOOL)]
        first_seg = self.nseg == 0
        self.nseg += 1

        def run(engname, eng):
            waited = self.waited[engname]

            def wait(sem, val, key):
                if val <= 0 or waited.get(key, 0) >= val:
                    return
                eng.wait_ge(sem, val)
                waited[key] = val

            if not first_seg:
                for key, sem, val in barrier:
                    if key == ("e", engname):
                        continue
                    wait(sem, val, key)
            for i, o in enumerate(ops):
                if o["eng"] != engname:
                    continue
                for d in sorted(o["deps"]):
                    od = ops[d]
                    if od["eng"] == "pe" and engname == "pe" and not od["dma"]:
                        continue
                    sem, val = od["done"]
                    wait(sem, val, od["key"])
                if o["dma"]:
                    wait(o["prev"][0], o["prev"][1], o["key"])
                ins = o["fn"](eng)
                if o["done"] is not None:
                    ins.then_inc(o["done"][0], 16 if o["dma"] else 1)
            if last and engname == "sp":
                for key, sem, val in final:
                    wait(sem, val, key)

        with nc.Block() as block:
            @block.sync
            def _(e):
                run("sp", e)

            @block.gpsimd
            def _(e):
                run("pool", e)

            @block.scalar
            def _(e):
                run("act", e)

            @block.vector
            def _(e):
                run("dve", e)

            @block.tensor
            def _(e):
                run("pe", e)
        self.reset()

    def close(self):
        self.st.close()


def build(NT, OWN, NE, CAP, debug=False):
    JH = NT // 2
    NCC = (CAP + 127) // 128
    LC = CAP - 128 * (NCC - 1)

    def CM(c):
        return 128 if c < NCC - 1 else LC
    KSEL = float(2 * NT * 128 // NE)
    nc = bass.Bass("TRN2", target_bir_lowering=False)

    def IN(name, shape, dt=F32):
        return nc.dram_tensor(name, shape, dt, kind="ExternalInput").ap()

    def SCR(name, shape, dt):
        return nc.dram_tensor(name, shape, dt).ap()

    xg = IN("xg", [NT * 128, D])
    memA = IN("memA", [NM, D])
    memB = IN("memB", [NM, D])
    w_in = IN("w_in", [D, DIN])
    w_spT = IN("w_spT", [NH, 128, 128])
    bsp_i = IN("bsp", [128, NH])
    lng_i = IN("lng", [128, DG])
    lnb_i = IN("lnb", [128, DG])
    wf_i = IN("w_fourier", [NH, 128, 128])
    goa_i = IN("goa", [128, DG])
    gob_i = IN("gob", [128, DG])
    w_out = IN("w_out", [D, D])
    gmix_i = IN("gmix", [128, D])
    gattn_i = IN("gattn", [128, D])
    gmem_i = IN("gmem", [128, D])
    gffn_i = IN("gffn", [128, D])
    gfin_i = IN("gfin", [128, D])
    w_q = IN("w_q", [D, D])
    w_kv = IN("w_kv", [D, 2 * D])
    w_o = IN("w_o", [D, D])
    w_r = IN("w_router", [D, NE])
    br_i = IN("br", [128, NE])
    w_gate = IN("w_gate", [NE, D, DE])
    w_up = IN("w_up", [NE, D, DE])
    w_down = IN("w_down", [NE, DE, D])
    cc_i = IN("cc", [128, 128])
    sc_i = IN("sc", [128, 128])
    c1_i = IN("c1", [NT, 2, 3, 128])
    m2re_i = IN("m2re", [128, 2 * 128 * JH])
    m2im_i = IN("m2im", [128, 2 * 128 * JH])
    ident_i = IN("ident", [128, 128])
    ones_i = IN("ones", [128, 128])
    tri_i = IN("tri", [128, 128])
    iota_i = IN("iota", [128, CAP])
    y_out = nc.dram_tensor("y", [OWN * 128, D], F32, kind="ExternalOutput").ap()
    dbg = {}
    if debug:
        for nm, shp in [("d_h1", [NT * 128, D]), ("d_h2", [OWN * 128, D]), ("d_aff", [NT * 128, NE]),
                        ("d_yb", [NT * 128, DG]), ("d_thr", [128, NE])]:
            dbg[nm] = nc.dram_tensor(nm, shp, F32, kind="ExternalOutput").ap()

    ya_d = SCR("ya_d", [NT * 128, DG], BF16)
    ab_d = SCR("ab_d", [NT * 128, 2 * DG], BF16)
    g_d = SCR("g_d", [2, 128, 128, 2, DG], BF16)
    yb_d = dbg["d_yb"] if debug else SCR("yb_d", [NT * 128, DG], F32)
    h1_d = dbg["d_h1"] if debug else SCR("h1_d", [NT * 128, D], F32)
    o_d = SCR("o_d", [NT * 128, D], BF16)
    h2_d = dbg["d_h2"] if debug else SCR("h2_d", [OWN * 128, D], F32)
    aff_d = dbg["d_aff"] if debug else SCR("aff_d", [NT * 128, NE], F32)
    xn3_d = SCR("xn3_d", [OWN * 128, D], BF16)
    yex_d = SCR("yex_d", [NE, CAP, D], BF16)

    top = contextlib.ExitStack()
    b = Bld(nc)

    uid = [0]

    def SBt(st, name, shape, dt=F32):
        uid[0] += 1
        return st.enter_context(nc.sbuf_tensor("%s_%d" % (name, uid[0]), shape, dt))

    ptr = [top.enter_context(nc.psum_tensor("ptr%d" % i, [128, 1024], BF16)) for i in range(2)]
    pf = [top.enter_context(nc.psum_tensor("pf%d" % i, [128, 512], F32)) for i in range(6)]
    PTR = ["ptr0", "ptr1"]
    PF = ["pf%d" % i for i in range(6)]
    ident = SBt(top, "ident", [128, 128], BF16)
    identf = SBt(top, "identf", [128, 128], F32)
    ones = SBt(top, "ones", [128, 128], BF16)
    tri = SBt(top, "tri", [128, 128], BF16)
    iota = SBt(top, "iota", [128, CAP], F32)
    ss = SBt(top, "ss", [128, 16], F32)
    b.dma("pool", ident[:], ident_i, w=["ident"])
    b.dma("sp", identf[:], ident_i, w=["identf"])
    b.dma("pool", ones[:], ones_i, w=["ones"])
    b.dma("pool", tri[:], tri_i, w=["tri"])
    b.dma("sp", iota[:], iota_i, w=["iota"])

    cpy_rr = [0]

    def evac(out, in_, r, w, scale=None):
        cpy_rr[0] += 1
        if scale is not None or cpy_rr[0] % 2 == 0:
            if scale is None:
                b.op("act", lambda e: e.copy(out=out, in_=in_), r=r, w=w)
            else:
                b.op("act", lambda e: e.activation(out=out, in_=in_, func=AF.Copy, scale=scale), r=r, w=w)
        else:
            b.op("dve", lambda e: e.tensor_copy(out=out, in_=in_), r=r, w=w)

    def rstd_of(src, n, eps, rkey, col, skey, scr, scrkey):
        sc = ss[:, col:col + 1]
        b.op("act", lambda e: e.activation(out=scr, in_=src, func=AF.Square, accum_out=sc),
             r=[rkey], w=[scrkey, skey])
        b.op("act", lambda e: e.activation(out=sc, in_=sc, func=AF.Sqrt, bias=float(eps), scale=1.0 / n),
             r=[skey], w=[skey])
        b.op("dve", lambda e: e.reciprocal(out=sc, in_=sc), r=[skey], w=[skey])
        return sc

    def transposes(src, skey, dst, dkey, n, idt=None, fp32=False, fbase=0):
        if not fp32:
            for g0 in range(0, n, 8):
                bank = (g0 // 8) % 2
                gn = min(8, n - g0)
                for i in range(gn):
                    b.op("pe", lambda e, i=i, g0=g0, bank=bank: e.transpose(
                        ptr[bank][:, i * 128:(i + 1) * 128], src[:, (g0 + i) * 128:(g0 + i + 1) * 128], ident[:]),
                        r=[skey, "ident"], w=[PTR[bank]])
                evac(dst[:, g0:g0 + gn, :], ptr[bank][:, 0:gn * 128].rearrange("p (a c) -> p a c", a=gn),
                     r=[PTR[bank]], w=[dkey])
        else:
            for g0 in range(0, n, 4):
                bank = fbase + (g0 // 4) % 2
                for i in range(4):
                    b.op("pe", lambda e, i=i, g0=g0, bank=bank: e.transpose(
                        pf[bank][:, i * 128:(i + 1) * 128], src[:, (g0 + i) * 128:(g0 + i + 1) * 128], identf[:]),
                        r=[skey, "identf"], w=[PF[bank]])
                evac(dst[:, g0:g0 + 4, :], pf[bank][:, :].rearrange("p (a c) -> p a c", a=4),
                     r=[PF[bank]], w=[dkey])

    def load_w(st, name, src, ncols, q="pool"):
        t = SBt(st, name, [128, KD, ncols], BF16)
        v = src.rearrange("(kc p) n -> p kc n", p=128)
        for kc in range(KD):
            b.dma(q, t[:, kc, :], v[:, kc, :], w=[name])
        return t

    def proj(xT, xkey, w_sb, wkey, ncols, sink, base=0):
        for ch in range(ncols // 512):
            k = base + ch % 2
            for kc in range(KD):
                b.op("pe", lambda e, k=k, kc=kc, ch=ch: e.matmul(
                    pf[k][:, :], lhsT=xT[:, kc, :], rhs=w_sb[:, kc, ch * 512:(ch + 1) * 512],
                    start=(kc == 0), stop=(kc == KD - 1)), r=[xkey, wkey], w=[PF[k]])
            sink(ch, pf[k], PF[k])

    st = contextlib.ExitStack()
    st0 = contextlib.ExitStack()
    cswf = SBt(st, "cswf", [128, NH, 256], BF16)
    wf = SBt(st0, "wf", [128, NH, 128], F32)
    ccf = SBt(st0, "ccf", [128, 128], F32)
    scf = SBt(st0, "scf", [128, 128], F32)
    b.dma("sp", wf[:], wf_i.rearrange("g c e -> c g e"), w=["wf"])
    b.dma("sp", ccf[:], cc_i, w=["ccf"])
    b.dma("sp", scf[:], sc_i, w=["scf"])
    for ci, (cm, ck) in enumerate([(ccf, "ccf"), (scf, "scf")]):
        for hf in range(2):
            k = ci * 2 + hf
            b.op("pe", lambda e, cm=cm, hf=hf, k=k: e.matmul(
                pf[k][:, :], lhsT=cm[:], rhs=wf[:].rearrange("p g e -> p (g e)")[:, hf * 512:(hf + 1) * 512], start=True, stop=True),
                r=[ck, "wf"], w=[PF[k]])
            evac(cswf[:, hf * 4:(hf + 1) * 4, ci * 128:(ci + 1) * 128],
                 pf[k][:, :].rearrange("p (g e) -> p g e", g=4), r=[PF[k]], w=["cswf"])
    b.flush()
    st0.close()
    win = load_w(st, "win", w_in, DIN)
    wsT = SBt(st, "wsT", [128, NH, 128], BF16)
    b.dma("pool", wsT[:], w_spT.rearrange("h q p -> q h p"), w=["wsT"])
    gmix = SBt(st, "gmix", [128, D]); b.dma("sp", gmix[:], gmix_i, w=["gmix"])
    lng = SBt(st, "lng", [128, DG]); b.dma("sp", lng[:], lng_i, w=["lng"])
    lnb = SBt(st, "lnb", [128, DG]); b.dma("sp", lnb[:], lnb_i, w=["lnb"])
    goa = SBt(st, "goa", [128, DG]); b.dma("sp", goa[:], goa_i, w=["goa"])
    bsp = SBt(st, "bspt", [128, NH]); b.dma("sp", bsp[:], bsp_i, w=["bsp"])
    xt = [SBt(st, "xt%d" % i, [128, D]) for i in range(2)]
    xn = [SBt(st, "xn%d" % i, [128, D], BF16) for i in range(2)]
    xnT = [SBt(st, "xnT%d" % i, [128, KD, 128], BF16) for i in range(2)]
    u = [SBt(st, "u%d" % i, [128, DG], BF16) for i in range(2)]
    v = [SBt(st, "v%d" % i, [128, DG], BF16) for i in range(2)]
    pb = [SBt(st, "pb%d" % i, [128, DG], BF16) for i in range(2)]
    t1 = SBt(st, "t1", [128, DG])
    vn = SBt(st, "vn", [128, DG], BF16)
    gt = t1
    ya0 = SBt(st, "ya0", [128, DG], BF16)
    pbT = SBt(st, "pbT", [128, NH, 128], BF16)
    ab0 = SBt(st, "ab0", [128, 2 * DG], BF16)
    st8 = SBt(st, "st8", [128, 4, NH])

    def v3(t):
        return t[:].rearrange("p (h d) -> p h d", h=NH)

    def b8(col):
        return st8[:, col, :, None].broadcast_to([128, NH, 128])

    def p1_S0(T):
        i2 = T % 2
        X = xt[i2]; xk = "xt%d" % i2
        XN = xn[i2]; xnk = "xn%d" % i2
        b.dma("sp", X[:], xg[T * 128:(T + 1) * 128, :], w=[xk])
        rs = rstd_of(X[:], D, EPS, xk, i2, "ssx%d" % i2, XN[:], xnk)
        b.op("dve", lambda e: e.scalar_tensor_tensor(out=XN[:], in0=X[:], scalar=rs, in1=gmix[:],
                                                     op0=ALU.mult, op1=ALU.mult),
             r=[xk, "ssx%d" % i2, "gmix"], w=[xnk])

    def p1_S1(T):
        i2 = T % 2
        transposes(xn[i2], "xn%d" % i2, xnT[i2], "xnT%d" % i2, KD)

    def p1_S2(T):
        i2 = T % 2
        U = u[i2]; V = v[i2]; PB = pb[i2]
        uk = "u%d" % i2; vk = "v%d" % i2; pbk = "pb%d" % i2

        def sink1(ch, ps, pk):
            if ch < 2:
                b.op("act", lambda e: e.activation(out=U[:, ch * 512:(ch + 1) * 512], in_=ps[:, :],
                                                   func=AF.Gelu_apprx_tanh), r=[pk], w=[uk])
            elif ch < 4:
                b.op("act", lambda e: e.activation(out=V[:, (ch - 2) * 512:(ch - 1) * 512], in_=ps[:, :],
                                                   func=AF.Gelu_apprx_tanh), r=[pk], w=[vk])
            else:
                b.op("act", lambda e: e.copy(out=PB[:, (ch - 4) * 512:(ch - 3) * 512], in_=ps[:, :]),
                     r=[pk], w=[pbk])
        proj(xnT[i2], "xnT%d" % i2, win, "win", DIN, sink1)

    def p1_S3a(T):
        i2 = T % 2
        V = v[i2]; vk = "v%d" % i2
        b.op("dve", lambda e: e.tensor_reduce(out=st8[:, 0, :], in_=v3(V), axis=AX.X, op=ALU.add), r=[vk], w=["st8a"])
        b.op("act", lambda e: e.activation(out=t1[:], in_=V[:], func=AF.Square), r=[vk], w=["t1"])
        b.op("dve", lambda e: e.tensor_reduce(out=st8[:, 1, :], in_=v3(t1), axis=AX.X, op=ALU.add), r=["t1"], w=["st8b"])
        b.op("dve", lambda e: e.tensor_scalar(out=st8[:, 0, :], in0=st8[:, 0, :], scalar1=1.0 / 128, scalar2=None,
                                              op0=ALU.mult), r=["st8a"], w=["st8a"])
        b.op("dve", lambda e: e.tensor_tensor(out=st8[:, 2, :], in0=st8[:, 0, :], in1=st8[:, 0, :], op=ALU.mult),
             r=["st8a"], w=["st8c"])
        b.op("dve", lambda e: e.scalar_tensor_tensor(out=st8[:, 2, :], in0=st8[:, 1, :], scalar=1.0 / 128,
                                                     in1=st8[:, 2, :], op0=ALU.mult, op1=ALU.subtract),
             r=["st8b", "st8c"], w=["st8c"])
        b.op("act", lambda e: e.activation(out=st8[:, 3, :], in_=st8[:, 2, :], func=AF.Sqrt, bias=float(LN_EPS),
                                           scale=1.0), r=["st8c"], w=["st8d"])
        b.op("dve", lambda e: e.reciprocal(out=st8[:, 3, :], in_=st8[:, 3, :]), r=["st8d"], w=["st8d"])
        b.op("dve", lambda e: e.tensor_tensor(out=v3(t1), in0=v3(V), in1=b8(0), op=ALU.subtract),
             r=[vk, "st8a"], w=["t1"])
        b.op("dve", lambda e: e.tensor_tensor(out=v3(t1), in0=v3(t1), in1=b8(3), op=ALU.mult),
             r=["t1", "st8d"], w=["t1"])
        b.op("dve", lambda e: e.tensor_tensor(out=t1[:], in0=t1[:], in1=lng[:], op=ALU.mult), r=["t1", "lng"], w=["t1"])
        b.op("dve", lambda e: e.tensor_tensor(out=vn[:], in0=t1[:], in1=lnb[:], op=ALU.add), r=["t1", "lnb"], w=["vn"])

    def p1_S3b(T):
        i2 = T % 2
        U = u[i2]; PB = pb[i2]
        uk = "u%d" % i2; pbk = "pb%d" % i2
        transposes(PB, pbk, pbT, "pbT", NH)
        for hf in range(2):
            for gl in range(4):
                g = hf * 4 + gl
                k = 4 + gl // 2
                b.op("pe", lambda e, g=g, gl=gl, k=k: e.matmul(pf[k][:, (gl % 2) * 256:(gl % 2 + 1) * 256],
                                                               lhsT=pbT[:, g, :], rhs=cswf[:, g, :], start=True, stop=True),
                     r=["pbT", "cswf"], w=[PF[k]])
                if gl % 2 == 1:
                    g0 = g - 1
                    evac(ab0[:].rearrange("p (r g e) -> p r g e", r=2, g=NH)[:, :, g0:g0 + 2, :],
                         pf[k][:, :].rearrange("p (g r e) -> p r g e", g=2, r=2), r=[PF[k]], w=["ab0"])
        b.dma("pool", ab_d[T * 128:(T + 1) * 128, :], ab0[:], r=["ab0"], w=[("ab_d", T)])
        for h in range(NH):
            k = 2 + h // 4
            b.op("pe", lambda e, h=h, k=k: e.matmul(pf[k][:, (h % 4) * 128:(h % 4 + 1) * 128], lhsT=wsT[:, h, :],
                                                    rhs=vn[:, h * 128:(h + 1) * 128], start=True, stop=True),
                 r=["wsT", "vn"], w=[PF[k]])
        for hf in range(2):
            b.op("dve", lambda e, hf=hf: e.tensor_tensor(
                out=gt[:, hf * 512:(hf + 1) * 512].rearrange("p (h d) -> p h d", h=4),
                in0=pf[2 + hf][:, :].rearrange("p (h d) -> p h d", h=4),
                in1=bsp[:, hf * 4:(hf + 1) * 4, None].broadcast_to([128, 4, 128]), op=ALU.add),
                r=[PF[2 + hf], "bsp"], w=["t1"])
        b.op("dve", lambda e: e.tensor_tensor(out=gt[:], in0=gt[:], in1=U[:], op=ALU.mult), r=["t1", uk], w=["t1"])
        rs2 = rstd_of(gt[:], DG, EPS, "t1", 2, "ss2", vn[:], "vn")
        b.op("dve", lambda e: e.scalar_tensor_tensor(out=ya0[:], in0=gt[:], scalar=rs2, in1=goa[:],
                                                     op0=ALU.mult, op1=ALU.mult),
             r=["t1", "ss2", "goa"], w=["ya0"])
        b.dma("pool", ya_d[T * 128:(T + 1) * 128, :], ya0[:], r=["ya0"], w=[("ya_d", T)])

    p1_S0(0)
    for i in range(NT + 2):
        if i + 1 < NT:
            p1_S0(i + 1)
        if i < NT:
            p1_S1(i)
        if 2 <= i:
            p1_S3a(i - 2)
        if 1 <= i <= NT:
            p1_S2(i - 1)
        if 2 <= i:
            p1_S3b(i - 2)
    b.flush()
    st.close()

    st = contextlib.ExitStack()
    FB = 4
    c1 = SBt(st, "c1", [NT, 2, 3, 128], BF16)
    b.dma("pool", c1[:], c1_i, w=["c1"])
    dat = [SBt(st, "dat%d" % i, [NT, FB, 2 * DG], BF16) for i in range(2)]
    gs = [SBt(st, "gs%d" % i, [128, 2, FB, 2, DG], BF16) for i in range(2)]
    abv = ab_d.rearrange("(p f) e -> p f e", f=128)
    rr = 0
    for fb in range(128 // FB):
        DT = dat[fb % 2]; dk = "dat%d" % (fb % 2)
        GS = gs[fb % 2]; gk = "gs%d" % (fb % 2)
        b.dma("sp", DT[:], abv[:, fb * FB:(fb + 1) * FB, :], w=[dk])
        for fl in range(FB):
            for m in range(2):
                for eh in range(2):
                    for ri in range(2):
                        k = rr % 6; rr += 1
                        l0, l1 = (0, 1) if ri == 0 else (1, 2)
                        b.op("pe", lambda e, k=k, m=m, l0=l0, fl=fl, eh=eh, DT=DT: e.matmul(
                            pf[k][:, :], lhsT=c1[:, m, l0, :], rhs=DT[:, fl, eh * 512:(eh + 1) * 512],
                            start=True, stop=False), r=["c1", dk], w=[PF[k]])
                        b.op("pe", lambda e, k=k, m=m, l1=l1, fl=fl, eh=eh, DT=DT: e.matmul(
                            pf[k][:, :], lhsT=c1[:, m, l1, :], rhs=DT[:, fl, DG + eh * 512:DG + (eh + 1) * 512],
                            start=False, stop=True), r=["c1", dk], w=[PF[k]])
                        evac(GS[:, m, fl, ri, eh * 512:(eh + 1) * 512], pf[k][:, :], r=[PF[k]], w=[gk])
        for m in range(2):
            b.dma("pool", g_d[m, :, fb * FB:(fb + 1) * FB, :, :], GS[:, m, :, :, :], r=[gk], w=[("g_d", m, fb)])
    b.flush()
    st.close()

    stW = contextlib.ExitStack()
    wout = load_w(stW, "wout", w_out, D)
    st = contextlib.ExitStack()
    QB = 4
    m2re = SBt(st, "m2re", [128, 2, 128, JH], BF16)
    m2im = SBt(st, "m2im", [128, 2, 128, JH], BF16)
    b.dma("pool", m2re[:].rearrange("p m q j -> p (m q j)"), m2re_i, w=["m2re"])
    b.dma("pool", m2im[:].rearrange("p m q j -> p (m q j)"), m2im_i, w=["m2im"])
    gin = [SBt(st, "gin%d" % i, [128, QB, 2, DG], BF16) for i in range(2)]
    yst = [SBt(st, "yst%d" % i, [JH, QB, DG], F32) for i in range(2)]
    ybv = yb_d.rearrange("(m j q) e -> m j q e", m=2, j=JH)
    it = 0
    for m in range(2):
        for qb in range(128 // QB):
            GI = gin[it % 2]; gik = "gin%d" % (it % 2)
            YS = yst[it % 2]; ysk = "yst%d" % (it % 2)
            it += 1
            b.dma("sp", GI[:], g_d[m, qb * QB:(qb + 1) * QB, :, :, :].rearrange("q f r e -> f q r e"),
                  r=[("g_d", m, fb) for fb in range(128 // FB)], w=[gik])
            for ql in range(QB):
                q = qb * QB + ql
                for eh in range(2):
                    k = rr % 6; rr += 1
                    b.op("pe", lambda e, k=k, m=m, q=q, ql=ql, eh=eh, GI=GI: e.matmul(
                        pf[k][0:JH, :], lhsT=m2re[:, m, q, :], rhs=GI[:, ql, 0, eh * 512:(eh + 1) * 512],
                        start=True, stop=False), r=["m2re", gik], w=[PF[k]])
                    b.op("pe", lambda e, k=k, m=m, q=q, ql=ql, eh=eh, GI=GI: e.matmul(
                        pf[k][0:JH, :], lhsT=m2im[:, m, q, :], rhs=GI[:, ql, 1, eh * 512:(eh + 1) * 512],
                        start=False, stop=True), r=["m2im", gik], w=[PF[k]])
                    evac(YS[:, ql, eh * 512:(eh + 1) * 512], pf[k][0:JH, :], r=[PF[k]], w=[ysk])
            b.dma("pool", ybv[m, :, qb * QB:(qb + 1) * QB, :], YS[:], r=[ysk], w=[("yb_d", m, qb)])
    b.flush()
    st.close()

    st = contextlib.ExitStack()
    gob = SBt(st, "gob", [128, DG]); b.dma("sp", gob[:], gob_i, w=["gob"])
    xt = [SBt(st, "xt%d" % i, [128, D]) for i in range(3)]
    ybt = [SBt(st, "ybt%d" % i, [128, DG]) for i in range(2)]
    cat = [SBt(st, "cat%d" % i, [128, D], BF16) for i in range(2)]
    catT = [SBt(st, "catT%d" % i, [128, KD, 128], BF16) for i in range(2)]
    h1 = [SBt(st, "h1_%d" % i, [128, D]) for i in range(2)]

    def a_S0(T):
        i2 = T % 2; i3 = T % 3
        rows = slice(T * 128, (T + 1) * 128)
        b.dma("sp", xt[i3][:], xg[rows, :], w=["xt%d" % i3])
        b.dma("sp", ybt[i2][:], yb_d[rows, :], w=["ybt%d" % i2])
        b.dma("sp", cat[i2][:, 0:DG], ya_d[rows, :], w=["cat%d" % i2])
        rs = rstd_of(ybt[i2][:], DG, EPS, "ybt%d" % i2, i2, "ssy%d" % i2, cat[i2][:, DG:D], "cat%d" % i2)
        b.op("dve", lambda e: e.scalar_tensor_tensor(out=cat[i2][:, DG:D], in0=ybt[i2][:], scalar=rs,
                                                     in1=gob[:], op0=ALU.mult, op1=ALU.mult),
             r=["ybt%d" % i2, "ssy%d" % i2, "gob"], w=["cat%d" % i2])

    def a_S1(T):
        i2 = T % 2
        transposes(cat[i2], "cat%d" % i2, catT[i2], "catT%d" % i2, KD)

    def a_S2(T):
        i2 = T % 2; i3 = T % 3
        rows = slice(T * 128, (T + 1) * 128)

        def sink2(ch, ps, pk):
            b.op("dve", lambda e: e.tensor_tensor(out=h1[i2][:, ch * 512:(ch + 1) * 512], in0=ps[:, :],
                                                  in1=xt[i3][:, ch * 512:(ch + 1) * 512], op=ALU.add),
                 r=[pk, "xt%d" % i3], w=["h1_%d" % i2])
        proj(catT[i2], "catT%d" % i2, wout, "wout", D, sink2)
        b.dma("pool", h1_d[rows, :], h1[i2][:], r=["h1_%d" % i2], w=[("h1_d", T)])

    a_S0(0)
    for i in range(NT + 1):
        if i + 1 < NT:
            a_S0(i + 1)
        if i < NT:
            a_S1(i)
        if i >= 1:
            a_S2(i - 1)
    b.flush()
    st.close()
    stW.close()

    st = contextlib.ExitStack()
    st0 = contextlib.ExitStack()
    kT = SBt(st, "kT", [128, 2, KD, NM], BF16)
    vsb = SBt(st, "vsb", [128, 2, 2, D], BF16)
    wq = load_w(st, "wq", w_q, D)
    gmem = SBt(st0, "gmem", [128, D]); b.dma("sp", gmem[:], gmem_i, w=["gmem"])
    memT = SBt(st0, "memT", [128, 2, KD, NM], BF16)
    mt = SBt(st0, "mt", [128, D])
    mn = SBt(st0, "mn", [128, D], BF16)
    mT1 = SBt(st0, "mT1", [128, KD, 128], BF16)
    wkv = [SBt(st0, "wkv%d" % i, [128, KD, 512], BF16) for i in range(2)]
    for a, msrc in enumerate([memA, memB]):
        for c in range(2):
            b.dma("sp", mt[:], msrc[c * 128:(c + 1) * 128, :], w=["mt"])
            rs = rstd_of(mt[:], D, EPS, "mt", 0, "ss0", mn[:], "mn")
            b.op("dve", lambda e, rs=rs: e.scalar_tensor_tensor(out=mn[:], in0=mt[:], scalar=rs, in1=gmem[:],
                                                                op0=ALU.mult, op1=ALU.mult),
                 r=["mt", "ss0", "gmem"], w=["mn"])
            transposes(mn, "mn", mT1, "mT1", KD)
            b.op("dve", lambda e, a=a, c=c: e.tensor_copy(out=memT[:, a, :, c * 128:(c + 1) * 128], in_=mT1[:]),
                 r=["mT1"], w=["memT"])
    wkvv = w_kv.rearrange("(kc p) n -> p kc n", p=128)
    for ch in range(8):
        WK = wkv[ch % 2]; wk = "wkv%d" % (ch % 2)
        b.dma("pool", WK[:], wkvv[:, :, ch * 512:(ch + 1) * 512], w=[wk])
        for a in range(2):
            if ch < 4:
                for dl in range(4):
                    dc = ch * 4 + dl
                    k = rr % 6; rr += 1
                    for kc in range(KD):
                        b.op("pe", lambda e, k=k, kc=kc, dl=dl, a=a, WK=WK: e.matmul(
                            pf[k][:, 0:NM], lhsT=WK[:, kc, dl * 128:(dl + 1) * 128], rhs=memT[:, a, kc, :],
                            start=(kc == 0), stop=(kc == KD - 1)), r=[wk, "memT"], w=[PF[k]])
                    evac(kT[:, a, dc, :], pf[k][:, 0:NM], r=[PF[k]], w=["kT"])
            else:
                for c in range(2):
                    k = rr % 6; rr += 1
                    for kc in range(KD):
                        b.op("pe", lambda e, k=k, kc=kc, c=c, a=a, WK=WK: e.matmul(
                            pf[k][:, :], lhsT=memT[:, a, kc, c * 128:(c + 1) * 128], rhs=WK[:, kc, :],
                            start=(kc == 0), stop=(kc == KD - 1)), r=[wk, "memT"], w=[PF[k]])
                    evac(vsb[:, a, c, (ch - 4) * 512:(ch - 3) * 512], pf[k][:, :], r=[PF[k]], w=["vsb"])
    b.flush()
    st0.close()
    gattn = SBt(st, "gattn", [128, D]); b.dma("sp", gattn[:], gattn_i, w=["gattn"])
    h1t = [SBt(st, "h1t%d" % i, [128, D]) for i in range(2)]
    xn2 = [SBt(st, "xn2_%d" % i, [128, D], BF16) for i in range(2)]
    xn2T = [SBt(st, "xn2T%d" % i, [128, KD, 128], BF16) for i in range(2)]
    qs = [SBt(st, "qs%d" % i, [128, D], BF16) for i in range(2)]
    qT = SBt(st, "qT", [128, KD, 128], BF16)
    pr = [SBt(st, "pr%d" % i, [128, 4 * NM], BF16) for i in range(2)]
    prT = SBt(st, "prT", [128, 8, 128], BF16)
    osb = [SBt(st, "osb%d" % i, [128, D], BF16) for i in range(2)]
    sm = [SBt(st, "sm%d" % i, [128, 3, 4]) for i in range(2)]

    def b_S0(T):
        i2 = T % 2
        rows = slice(T * 128, (T + 1) * 128)
        H = h1t[i2]; hk = "h1t%d" % i2
        XN = xn2[i2]; xk = "xn2_%d" % i2
        b.dma("sp", H[:], h1_d[rows, :], w=[hk])
        rs = rstd_of(H[:], D, EPS, hk, i2, "ssh%d" % i2, XN[:], xk)
        b.op("dve", lambda e: e.scalar_tensor_tensor(out=XN[:], in0=H[:], scalar=rs, in1=gattn[:],
                                                     op0=ALU.mult, op1=ALU.mult),
             r=[hk, "ssh%d" % i2, "gattn"], w=[xk])

    def b_S1(T):
        i2 = T % 2
        transposes(xn2[i2], "xn2_%d" % i2, xn2T[i2], "xn2T%d" % i2, KD)

    def b_S2(T):
        i2 = T % 2
        Q = qs[i2]; qk = "qs%d" % i2

        def sink3(ch, ps, pk):
            b.op("act", lambda e: e.activation(out=Q[:, ch * 512:(ch + 1) * 512], in_=ps[:, :], func=AF.Copy,
                                               scale=float(512 ** -0.5)), r=[pk], w=[qk])
        proj(xn2T[i2], "xn2T%d" % i2, wq, "wq", D, sink3)

    def b_S3a(T):
        i2 = T % 2
        transposes(qs[i2], "qs%d" % i2, qT, "qT", KD)

    def b_S3b(T):
        i2 = T % 2
        a = 0 if T < NT // 2 else 1
        SM = sm[i2]; PR = pr[i2]
        s0 = "sm0_%d" % i2; s1 = "sm1_%d" % i2; s2 = "sm2_%d" % i2; prk = "pr%d" % i2
        for h in range(4):
            k = 2 + h // 2
            for c in range(4):
                b.op("pe", lambda e, h=h, c=c, k=k: e.matmul(
                    pf[k][:, (h % 2) * NM:(h % 2 + 1) * NM], lhsT=qT[:, 4 * h + c, :], rhs=kT[:, a, 4 * h + c, :],
                    start=(c == 0), stop=(c == 3)), r=["qT", "kT"], w=[PF[k]])
        for hp in range(2):
            b.op("dve", lambda e, hp=hp: e.tensor_reduce(out=SM[:, 0, hp * 2:hp * 2 + 2],
                                                         in_=pf[2 + hp][:, :].rearrange("p (h m) -> p h m", h=2),
                                                         axis=AX.X, op=ALU.max), r=[PF[2 + hp]], w=[s0])
        b.op("dve", lambda e: e.tensor_scalar(out=SM[:, 0, :], in0=SM[:, 0, :], scalar1=-1.0, scalar2=None,
                                              op0=ALU.mult), r=[s0], w=[s0])
        for h in range(4):
            b.op("act", lambda e, h=h: e.activation(out=PR[:, h * NM:(h + 1) * NM],
                                                    in_=pf[2 + h // 2][:, (h % 2) * NM:(h % 2 + 1) * NM], func=AF.Exp,
                                                    bias=SM[:, 0, h:h + 1], scale=1.0, accum_out=SM[:, 1, h:h + 1]),
                 r=[PF[2 + h // 2], s0], w=[prk, s1])
        b.op("dve", lambda e: e.reciprocal(out=SM[:, 2, :], in_=SM[:, 1, :]), r=[s1], w=[s2])

    def b_S4(T):
        i2 = T % 2
        a = 0 if T < NT // 2 else 1
        rows = slice(T * 128, (T + 1) * 128)
        SM = sm[i2]; PR = pr[i2]
        s2 = "sm2_%d" % i2; prk = "pr%d" % i2
        transposes(PR, prk, prT, "prT", 8)
        O = osb[i2]; ok = "osb%d" % i2
        for h in range(4):
            k = 4 + h % 2
            for c in range(2):
                b.op("pe", lambda e, h=h, c=c, k=k: e.matmul(
                    pf[k][:, :], lhsT=prT[:, 2 * h + c, :], rhs=vsb[:, a, c, h * 512:(h + 1) * 512],
                    start=(c == 0), stop=(c == 1)), r=["prT", "vsb"], w=[PF[k]])
            b.op("act", lambda e, h=h, k=k: e.activation(out=O[:, h * 512:(h + 1) * 512], in_=pf[k][:, :],
                                                         func=AF.Copy, scale=SM[:, 2, h:h + 1]),
                 r=[PF[k], s2], w=[ok])
        b.dma("pool", o_d[rows, :], O[:], r=[ok], w=[("o_d", T)])

    b_S0(0)
    for i in range(NT + 3):
        if i + 1 < NT:
            b_S0(i + 1)
        if i < NT:
            b_S1(i)
        if 2 <= i <= NT + 1:
            b_S3a(i - 2)
        if 1 <= i <= NT:
            b_S2(i - 1)
        if 2 <= i <= NT + 1:
            b_S3b(i - 2)
        if i >= 3:
            b_S4(i - 3)
    b.flush()
    st.close()

    st = contextlib.ExitStack()
    wo = load_w(st, "wo", w_o, D)
    gffn = SBt(st, "gffn", [128, D]); b.dma("sp", gffn[:], gffn_i, w=["gffn"])
    wr = SBt(st, "wr", [128, KD, NE]); b.dma("sp", wr[:], w_r.rearrange("(kc p) n -> p kc n", p=128), w=["wr"])
    br = SBt(st, "brt", [128, NE]); b.dma("sp", br[:], br_i, w=["br"])
    h1t = [SBt(st, "h1t%d" % i, [128, D]) for i in range(3)]
    ot = [SBt(st, "ot%d" % i, [128, D], BF16) for i in range(2)]
    oT = [SBt(st, "oT%d" % i, [128, KD, 128], BF16) for i in range(2)]
    h2 = [SBt(st, "h2_%d" % i, [128, D]) for i in range(2)]
    xn3 = [SBt(st, "xn3_%d" % i, [128, D]) for i in range(2)]
    xn3b = [SBt(st, "xn3b%d" % i, [128, D], BF16) for i in range(2)]
    xn3T = SBt(st, "xn3T", [128, KD, 128])
    lg = [SBt(st, "lg%d" % i, [128, NE]) for i in range(2)]
    sm = SBt(st, "sm", [128, 3])

    def c_S0(T):
        i2 = T % 2; i3 = T % 3
        rows = slice(T * 128, (T + 1) * 128)
        b.dma("sp", h1t[i3][:], h1_d[rows, :], w=["h1t%d" % i3])
        b.dma("sp", ot[i2][:], o_d[rows, :], w=["ot%d" % i2])

    def c_S1(T):
        i2 = T % 2
        transposes(ot[i2], "ot%d" % i2, oT[i2], "oT%d" % i2, KD)

    def c_S2(T):
        i2 = T % 2; i3 = T % 3
        rows = slice(T * 128, (T + 1) * 128)
        H = h1t[i3]; hk = "h1t%d" % i3
        H2 = h2[i2]; h2k = "h2_%d" % i2
        X3 = xn3[i2]; x3k = "xn3_%d" % i2

        def sink4(ch, ps, pk):
            b.op("dve", lambda e: e.tensor_tensor(out=H2[:, ch * 512:(ch + 1) * 512], in0=ps[:, :],
                                                  in1=H[:, ch * 512:(ch + 1) * 512], op=ALU.add),
                 r=[pk, hk], w=[h2k])
        proj(oT[i2], "oT%d" % i2, wo, "wo", D, sink4)
        if T < OWN:
            b.dma("pool", h2_d[rows, :], H2[:], r=[h2k], w=[("h2_d", T)])
        rs = rstd_of(H2[:], D, EPS, h2k, i2, "ssh%d" % i2, xn3b[i2][:], "xn3b%d" % i2)
        b.op("dve", lambda e: e.scalar_tensor_tensor(out=X3[:], in0=H2[:], scalar=rs, in1=gffn[:],
                                                     op0=ALU.mult, op1=ALU.mult),
             r=[h2k, "ssh%d" % i2, "gffn"], w=[x3k])
        if T < OWN:
            b.op("act", lambda e: e.copy(out=xn3b[i2][:], in_=X3[:]), r=[x3k], w=["xn3b%d" % i2])
            b.dma("pool", xn3_d[rows, :], xn3b[i2][:], r=["xn3b%d" % i2], w=[("xn3_d", T)])

    def c_S3(T):
        i2 = T % 2
        rows = slice(T * 128, (T + 1) * 128)
        transposes(xn3[i2], "xn3_%d" % i2, xn3T, "xn3T", KD, fp32=True, fbase=3)
        for kc in range(KD):
            b.op("pe", lambda e, kc=kc: e.matmul(pf[2][:, 0:NE], lhsT=xn3T[:, kc, :], rhs=wr[:, kc, :],
                                                 start=(kc == 0), stop=(kc == KD - 1)), r=["xn3T", "wr"], w=[PF[2]])
        Lg = lg[i2]; lk = "lg%d" % i2
        b.op("dve", lambda e: e.tensor_tensor(out=Lg[:], in0=pf[2][:, 0:NE], in1=br[:], op=ALU.add),
             r=[PF[2], "br"], w=[lk])
        b.op("dve", lambda e: e.tensor_reduce(out=sm[:, 0:1], in_=Lg[:], axis=AX.X, op=ALU.max), r=[lk], w=["sm0"])
        b.op("dve", lambda e: e.tensor_scalar(out=sm[:, 0:1], in0=sm[:, 0:1], scalar1=-1.0, scalar2=None, op0=ALU.mult),
             r=["sm0"], w=["sm0"])
        b.op("act", lambda e: e.activation(out=Lg[:], in_=Lg[:], func=AF.Exp, bias=sm[:, 0:1], scale=1.0,
                                           accum_out=sm[:, 1:2]), r=[lk, "sm0"], w=[lk, "sm1"])
        b.op("dve", lambda e: e.reciprocal(out=sm[:, 2:3], in_=sm[:, 1:2]), r=["sm1"], w=["sm2"])
        b.op("dve", lambda e: e.tensor_scalar(out=Lg[:], in0=Lg[:], scalar1=sm[:, 2:3], scalar2=None, op0=ALU.mult),
             r=[lk, "sm2"], w=[lk])
        b.dma("pool", aff_d[rows, :], Lg[:], r=[lk], w=[("aff_d", T)])

    c_S0(0)
    for i in range(NT + 2):
        if i + 1 < NT:
            c_S0(i + 1)
        if i < NT:
            c_S1(i)
        if 1 <= i <= NT:
            c_S2(i - 1)
        if i >= 2:
            c_S3(i - 2)
    b.flush()
    st.close()

    stB = contextlib.ExitStack()
    affg = SBt(stB, "affg", [128, NT, NE])
    b.dma("sp", affg[:], aff_d.rearrange("(t p) e -> p t e", p=128), w=["affg"])
    lo = SBt(stB, "lo", [128, NE]); hi = SBt(stB, "hi", [128, NE]); mid = SBt(stB, "mid", [128, NE])
    cmpt = SBt(stB, "cmpt", [128, NT, NE])
    cntp = SBt(stB, "cntp", [128, NE])
    onesf = SBt(stB, "onesf", [128, 128])
    b.dma("sp", onesf[:], ones_i, w=["onesf"])
    ge = SBt(stB, "ge", [128, NE]); tmp = SBt(stB, "tmp", [128, NE])
    sel = SBt(stB, "sel", [128, OWN, NE]); gate = SBt(stB, "gate", [128, OWN, NE]); slot = SBt(stB, "slot", [128, OWN, NE])
    selb = SBt(stB, "selb", [128, OWN, NE], BF16)
    off = SBt(stB, "off", [128, OWN, NE])
    b.op("dve", lambda e: e.memset(lo[:], 0.0), w=["lo"])
    b.op("dve", lambda e: e.memset(hi[:], 1.0), w=["hi"])
    b.op("dve", lambda e: e.memset(mid[:], 0.5), w=["mid"])
    for itn in range(34):
        b.op("dve", lambda e: e.tensor_tensor(out=cmpt[:], in0=affg[:], in1=mid[:, None, :].broadcast_to([128, NT, NE]),
                                              op=ALU.is_ge), r=["affg", "mid"], w=["cmpt"])
        b.op("dve", lambda e: e.tensor_reduce(out=cntp[:], in_=cmpt[:].rearrange("p t e -> p e t"), axis=AX.X, op=ALU.add),
             r=["cmpt"], w=["cntp"])
        b.op("pe", lambda e: e.matmul(pf[0][:, 0:NE], lhsT=onesf[:], rhs=cntp[:], start=True, stop=True),
             r=["onesf", "cntp"], w=[PF[0]])
        b.op("dve", lambda e: e.tensor_scalar(out=ge[:], in0=pf[0][:, 0:NE], scalar1=KSEL, scalar2=None, op0=ALU.is_ge),
             r=[PF[0]], w=["ge"])
        b.op("dve", lambda e: e.tensor_tensor(out=tmp[:], in0=ge[:], in1=mid[:], op=ALU.mult), r=["ge", "mid"], w=["tmp"])
        b.op("dve", lambda e: e.tensor_tensor(out=lo[:], in0=lo[:], in1=tmp[:], op=ALU.max), r=["lo", "tmp"], w=["lo"])
        b.op("dve", lambda e: e.scalar_tensor_tensor(out=tmp[:], in0=ge[:], scalar=4.0, in1=mid[:], op0=ALU.mult,
                                                     op1=ALU.add), r=["ge", "mid"], w=["tmp"])
        b.op("dve", lambda e: e.tensor_tensor(out=hi[:], in0=hi[:], in1=tmp[:], op=ALU.min), r=["hi", "tmp"], w=["hi"])
        b.op("dve", lambda e: e.tensor_tensor(out=mid[:], in0=lo[:], in1=hi[:], op=ALU.add), r=["lo", "hi"], w=["mid"])
        b.op("dve", lambda e: e.tensor_scalar(out=mid[:], in0=mid[:], scalar1=0.5, scalar2=None, op0=ALU.mult),
             r=["mid"], w=["mid"])
    if debug:
        b.dma("sp", dbg["d_thr"], lo[:], r=["lo"])
    b.op("dve", lambda e: e.tensor_tensor(out=sel[:], in0=affg[:, 0:OWN, :], in1=lo[:, None, :].broadcast_to([128, OWN, NE]),
                                          op=ALU.is_ge), r=["affg", "lo"], w=["sel"])
    b.op("dve", lambda e: e.tensor_tensor(out=gate[:], in0=affg[:, 0:OWN, :], in1=sel[:], op=ALU.mult),
         r=["affg", "sel"], w=["gate"])
    b.op("dve", lambda e: e.tensor_copy(out=selb[:], in_=sel[:]), r=["sel"], w=["selb"])
    sflat = selb[:].rearrange("p t e -> p (t e)")
    b.op("pe", lambda e: e.matmul(pf[1][:, 0:OWN * NE], lhsT=tri[:], rhs=sflat, start=True, stop=True),
         r=["tri", "selb"], w=[PF[1]])
    b.op("pe", lambda e: e.matmul(pf[2][:, 0:OWN * NE], lhsT=ones[:], rhs=sflat, start=True, stop=True),
         r=["ones", "selb"], w=[PF[2]])
    b.op("dve", lambda e: e.memset(off[:], 0.0), w=["off"])
    for T in range(1, OWN):
        b.op("dve", lambda e, T=T: e.tensor_tensor(out=off[:, T, :], in0=off[:, T - 1, :],
                                                   in1=pf[2][:, (T - 1) * NE:T * NE], op=ALU.add),
             r=["off", PF[2]], w=["off"])
    b.op("dve", lambda e: e.tensor_tensor(out=slot[:].rearrange("p t e -> p (t e)"), in0=pf[1][:, 0:OWN * NE],
                                          in1=off[:].rearrange("p t e -> p (t e)"), op=ALU.add),
         r=[PF[1], "off"], w=["slot"])
    st = contextlib.ExitStack()
    xn3s = SBt(st, "xn3s", [128, OWN, D], BF16)
    for T in range(OWN):
        b.dma("sp", xn3s[:, T, :], xn3_d[T * 128:(T + 1) * 128, :], w=["xn3s"])
    b.flush()

    Pt = [SBt(st, "Pt%d" % i, [128, CAP], BF16) for i in range(4)]
    xsT = SBt(st, "xsT", [128, KD, CAP], BF16)
    hT = SBt(st, "hT", [128, FC, CAP], BF16)
    FBW = 256
    wg = [SBt(st, "wg%d" % i, [128, KD, FBW], BF16) for i in range(2)]
    wu = [SBt(st, "wu%d" % i, [128, KD, FBW], BF16) for i in range(2)]
    WDB = 4
    wd = [SBt(st, "wd%d" % i, [128, WDB, 512], BF16) for i in range(2)]
    ysb = SBt(st, "ysb", [128, NCC, D], BF16)
    sg = [SBt(st, "sg%d" % i, [128, CAP]) for i in range(2)]
    wblk = 0
    dblk = 0
    for ex in range(NE):
        for dg in range(KD // 4):
            for T in range(OWN):
                P = Pt[T % 4]; pk = "Pt%d" % (T % 4)
                b.op("dve", lambda e, P=P, T=T, ex=ex: e.tensor_scalar(
                    out=P[:], in0=iota[:], scalar1=slot[:, T, ex:ex + 1], scalar2=sel[:, T, ex:ex + 1],
                    op0=ALU.is_equal, op1=ALU.mult), r=["iota", "slot", "sel"], w=[pk])
                for dl in range(4):
                    dc = dg * 4 + dl
                    b.op("pe", lambda e, P=P, T=T, dc=dc, dl=dl: e.matmul(
                        pf[dl][:, 0:CAP], lhsT=xn3s[:, T, dc * 128:(dc + 1) * 128], rhs=P[:],
                        start=(T == 0), stop=(T == OWN - 1)), r=["xn3s", pk], w=[PF[dl]])
            for dl in range(4):
                evac(xsT[:, dg * 4 + dl, :], pf[dl][:, 0:CAP], r=[PF[dl]], w=["xsT"])
        wgv = w_gate[ex].rearrange("(kc p) f -> p kc f", p=128)
        wuv = w_up[ex].rearrange("(kc p) f -> p kc f", p=128)
        for fb in range(DE // FBW):
            i2 = wblk % 2; wblk += 1
            b.dma("pool", wg[i2][:], wgv[:, :, fb * FBW:(fb + 1) * FBW], w=["wg%d" % i2])
            b.dma("pool", wu[i2][:], wuv[:, :, fb * FBW:(fb + 1) * FBW], w=["wu%d" % i2])
            for fl in range(FBW // 128):
                fc = fb * (FBW // 128) + fl
                kg, ku = (4, 5) if fc % 2 == 0 else (2, 3)
                for kc in range(KD):
                    b.op("pe", lambda e, kc=kc, fl=fl, i2=i2, kg=kg: e.matmul(
                        pf[kg][:, 0:CAP], lhsT=wg[i2][:, kc, fl * 128:(fl + 1) * 128], rhs=xsT[:, kc, :],
                        start=(kc == 0), stop=(kc == KD - 1)), r=["wg%d" % i2, "xsT"], w=[PF[kg]])
                for kc in range(KD):
                    b.op("pe", lambda e, kc=kc, fl=fl, i2=i2, ku=ku: e.matmul(
                        pf[ku][:, 0:CAP], lhsT=wu[i2][:, kc, fl * 128:(fl + 1) * 128], rhs=xsT[:, kc, :],
                        start=(kc == 0), stop=(kc == KD - 1)), r=["wu%d" % i2, "xsT"], w=[PF[ku]])
                S = sg[fc % 2]; sk = "sg%d" % (fc % 2)
                b.op("act", lambda e, S=S, kg=kg: e.activation(out=S[:], in_=pf[kg][:, 0:CAP], func=AF.Silu), r=[PF[kg]], w=[sk])
                b.op("dve", lambda e, S=S, fc=fc, ku=ku: e.tensor_tensor(out=hT[:, fc, :], in0=pf[ku][:, 0:CAP], in1=S[:],
                                                                         op=ALU.mult), r=[PF[ku], sk], w=["hT"])
        wdv = w_down[ex].rearrange("(fc p) d -> p fc d", p=128)
        for dch in range(4):
            for fcb in range(FC // WDB):
                i2 = dblk % 2; dblk += 1
                b.dma("pool", wd[i2][:], wdv[:, fcb * WDB:(fcb + 1) * WDB, dch * 512:(dch + 1) * 512], w=["wd%d" % i2])
                for fl in range(WDB):
                    fc = fcb * WDB + fl
                    for ncc in range(NCC):
                        kb = (dch % 2) * 3 + ncc
                        b.op("pe", lambda e, fc=fc, fl=fl, ncc=ncc, i2=i2, kb=kb: e.matmul(
                            pf[kb][0:CM(ncc), :], lhsT=hT[:, fc, ncc * 128:ncc * 128 + CM(ncc)], rhs=wd[i2][:, fl, :],
                            start=(fc == 0), stop=(fc == FC - 1)), r=["hT", "wd%d" % i2], w=[PF[kb]])
            for ncc in range(NCC):
                kb = (dch % 2) * 3 + ncc
                evac(ysb[0:CM(ncc), ncc, dch * 512:(dch + 1) * 512], pf[kb][0:CM(ncc), :], r=[PF[kb]], w=["ysb"])
        if NCC > 1:
            b.dma("sp", yex_d[ex, 0:(NCC - 1) * 128, :].rearrange("(c p) d -> p c d", p=128), ysb[:, 0:NCC - 1, :],
                  r=["ysb"], w=[("yex_d", ex, 0)])
        b.dma("sp", yex_d[ex, (NCC - 1) * 128:CAP, :], ysb[0:LC, NCC - 1, :], r=["ysb"], w=[("yex_d", ex, 1)])
    b.flush()
    st.close()

    st = contextlib.ExitStack()
    gfin = SBt(st, "gfin", [128, D]); b.dma("sp", gfin[:], gfin_i, w=["gfin"])
    Pg = [SBt(st, "Pg%d" % i, [128, CAP], BF16) for i in range(2)]
    PTg = [SBt(st, "PTg%d" % i, [128, NE, NCC, 128], BF16) for i in range(4)]
    yl = [SBt(st, "yl%d" % i, [128, NCC, D // 2], BF16) for i in range(3)]
    h2t = [SBt(st, "h2t%d" % i, [128, D]) for i in range(4)]
    h3 = [SBt(st, "h3_%d" % i, [128, D]) for i in range(2)]
    yo = [SBt(st, "yo%d" % i, [128, D]) for i in range(2)]
    yblk = 0
    pcount = 0
    for r in range(OWN // 2):
        tiles = [2 * r, 2 * r + 1]
        PTs = []
        for tl, T in enumerate(tiles):
            hb = (2 * r + tl) % 4
            b.dma("sp", h2t[hb][:], h2_d[T * 128:(T + 1) * 128, :], w=["h2t%d" % hb])
            PT = PTg[hb]; ptk = "PTg%d" % hb
            PTs.append((PT, ptk, hb))
            for ex in range(NE):
                P = Pg[pcount % 2]; pk = "Pg%d" % (pcount % 2)
                bank = pcount % 2
                pcount += 1
                b.op("dve", lambda e, P=P, T=T, ex=ex: e.tensor_scalar(
                    out=P[:], in0=iota[:], scalar1=slot[:, T, ex:ex + 1], scalar2=gate[:, T, ex:ex + 1],
                    op0=ALU.is_equal, op1=ALU.mult), r=["iota", "slot", "gate"], w=[pk])
                for c in range(NCC):
                    b.op("pe", lambda e, P=P, c=c, bank=bank: e.transpose(ptr[bank][0:CM(c), c * 128:(c + 1) * 128],
                                                                         P[:, c * 128:c * 128 + CM(c)], ident[:]),
                         r=[pk, "ident"], w=[PTR[bank]])
                if NCC > 1:
                    evac(PT[:, ex, 0:NCC - 1, :], ptr[bank][:, 0:(NCC - 1) * 128].rearrange("p (a c) -> p a c", a=NCC - 1),
                         r=[PTR[bank]], w=[ptk])
                evac(PT[0:LC, ex, NCC - 1, :], ptr[bank][0:LC, (NCC - 1) * 128:NCC * 128], r=[PTR[bank]], w=[ptk])
        for dh in range(2):
            for ex in range(NE):
                j3 = yblk % 3; yblk += 1
                dcols = slice(dh * (D // 2), (dh + 1) * (D // 2))
                if NCC > 1:
                    b.dma("sp", yl[j3][:, 0:NCC - 1, :],
                          yex_d[ex, 0:(NCC - 1) * 128, :].rearrange("(c p) d -> p c d", p=128)[:, :, dcols], w=["yl%d" % j3])
                b.dma("sp", yl[j3][0:LC, NCC - 1, :], yex_d[ex, (NCC - 1) * 128:CAP, dcols], w=["yl%d" % j3])
                for tl in range(2):
                    PT, ptk, hb = PTs[tl]
                    for dc in range(2):
                        k = tl * 2 + dc
                        for c in range(NCC):
                            b.op("pe", lambda e, ex=ex, c=c, dc=dc, j3=j3, PT=PT, k=k: e.matmul(
                                pf[k][:, :], lhsT=PT[0:CM(c), ex, c, :], rhs=yl[j3][0:CM(c), c, dc * 512:(dc + 1) * 512],
                                start=(ex == 0 and c == 0), stop=(ex == NE - 1 and c == NCC - 1)),
                                r=[ptk, "yl%d" % j3], w=[PF[k]])
            for tl in range(2):
                PT, ptk, hb = PTs[tl]
                for dc in range(2):
                    k = tl * 2 + dc
                    col = dh * (D // 2) + dc * 512
                    b.op("dve", lambda e, k=k, col=col, tl=tl, hb=hb: e.tensor_tensor(
                        out=h3[tl][:, col:col + 512], in0=pf[k][:, :], in1=h2t[hb][:, col:col + 512],
                        op=ALU.add), r=[PF[k], "h2t%d" % hb], w=["h3_%d" % tl])
        for tl, T in enumerate(tiles):
            rs = rstd_of(h3[tl][:], D, EPS, "h3_%d" % tl, tl, "ss%d" % tl, yo[tl][:], "yo%d" % tl)
            b.op("dve", lambda e, tl=tl, rs=rs: e.scalar_tensor_tensor(out=yo[tl][:], in0=h3[tl][:], scalar=rs, in1=gfin[:],
                                                                      op0=ALU.mult, op1=ALU.mult),
                 r=["h3_%d" % tl, "ss%d" % tl, "gfin"], w=["yo%d" % tl])
            b.dma("pool", y_out[T * 128:(T + 1) * 128, :], yo[tl][:], r=["yo%d" % tl], w=[("y", T)])
    b.flush(last=True)
    st.close()
    stB.close()
    b.close()
    top.close()
    return nc


def _tile_perm(x, order):
    return np.ascontiguousarray(x.reshape(-1, 128, x.shape[-1])[order].reshape(-1, x.shape[-1]))


def _dft_tables(order, NT, two_seq, bseq):
    JH = NT // 2
    order = np.asarray(order)
    q = np.arange(128)
    f = np.arange(128)
    if not two_seq:
        NTs = NT
        S = NT * 128
        tloc = order
        seq_of = np.zeros(NT, dtype=int)
        half_seq = [0, 0]
    else:
        NTs = NT // 2
        S = NTs * 128
        tloc = order % NTs
        seq_of = order // NTs
        half_seq = [bseq, 1 - bseq]
    c1 = np.zeros((NT, 2, 3, 128), dtype=np.float64)
    for m in range(2):
        ang = -2 * np.pi * np.outer(tloc, q) / NTs
        msk = (seq_of == half_seq[m]).astype(np.float64)[:, None]
        re = np.cos(ang) * msk
        im = np.sin(ang) * msk
        c1[:, m, 0, :] = re
        c1[:, m, 1, :] = im
        c1[:, m, 2, :] = -re
    m2re = np.zeros((128, 2, 128, JH), dtype=np.float64)
    m2im = np.zeros((128, 2, 128, JH), dtype=np.float64)
    for m in range(2):
        jj = tloc[m * JH:(m + 1) * JH]
        ang = -2 * np.pi * (f[:, None, None] * jj[None, None, :] / NTs + f[:, None, None] * q[None, :, None] / S)
        m2re[:, m] = np.cos(ang)
        m2im[:, m] = -np.sin(ang)
    c = np.arange(128)
    nrm = 1.0 / math.sqrt(S * 128.0)
    ang = 2 * np.pi * np.outer(c, c) / 128
    cc = np.cos(ang) * nrm
    sc = np.sin(ang) * nrm
    f32 = lambda a: np.ascontiguousarray(a.astype(np.float32))
    return dict(c1=f32(c1), m2re=f32(m2re.reshape(128, -1)), m2im=f32(m2im.reshape(128, -1)), cc=f32(cc), sc=f32(sc))


def _core_inputs(inp, NT, OWN, CAP, cfgs):
    rep = lambda vv: np.ascontiguousarray(np.broadcast_to(np.asarray(vv, np.float32).reshape(1, -1), (128, np.asarray(vv).size)))
    NE = inp["w_router"].shape[-1]
    shared = dict(
        w_in=np.ascontiguousarray(inp["w_in"][0]), w_spT=np.ascontiguousarray(np.transpose(inp["w_spatial"][0], (0, 2, 1))),
        bsp=np.ascontiguousarray(inp["b_spatial"][0].T), lng=rep(inp["ln_v_g"][0]), lnb=rep(inp["ln_v_b"][0]),
        w_fourier=np.ascontiguousarray(inp["w_fourier"][0]), goa=rep(inp["norm_out_a"][0]), gob=rep(inp["norm_out_b"][0]),
        w_out=np.ascontiguousarray(inp["w_out"][0]), gmix=rep(inp["norm_mix"][0]), gattn=rep(inp["norm_attn"][0]),
        gmem=rep(inp["norm_mem"][0]), gffn=rep(inp["norm_ffn"][0]), gfin=rep(inp["norm_final"]),
        w_q=np.ascontiguousarray(inp["w_q"][0]), w_kv=np.ascontiguousarray(inp["w_kv"][0]),
        w_o=np.ascontiguousarray(inp["w_o"][0]), w_router=np.ascontiguousarray(inp["w_router"][0]),
        br=rep(inp["b_router"][0]), w_gate=np.ascontiguousarray(inp["w_gate"][0]), w_up=np.ascontiguousarray(inp["w_up"][0]),
        w_down=np.ascontiguousarray(inp["w_down"][0]),
        ident=np.eye(128, dtype=np.float32), ones=np.ones((128, 128), np.float32),
        tri=np.triu(np.ones((128, 128), np.float32), 1),
        iota=np.ascontiguousarray(np.broadcast_to(np.arange(CAP, dtype=np.float32)[None, :], (128, CAP))),
    )
    maps = []
    for (xgrp, order, two_seq, bseq, mA, mB) in cfgs:
        m = dict(shared)
        m["xg"] = _tile_perm(xgrp, order)
        m["memA"] = np.ascontiguousarray(mA)
        m["memB"] = np.ascontiguousarray(mB)
        m.update(_dft_tables(order, NT, two_seq, bseq))
        maps.append(m)
    return maps


_NC_CACHE = {}


def kernel(**inp):
    inp = {k: np.asarray(v) for k, v in inp.items()}
    NT, OWN, CAP = 64, 16, 320
    NE = inp["w_router"].shape[-1]
    xp = inp["x_prompt"][0]
    xs = inp["x_sample"].reshape(-1, D)
    cfgs = []
    orders = []
    for c in range(4):
        own = list(range(16 * c, 16 * c + 16))
        order = own + [t for t in range(NT) if t not in own]
        orders.append(order)
        cfgs.append((xp, order, False, 0, inp["mem_prompt"][0], inp["mem_prompt"][0]))
    for cs in range(4):
        bq = cs // 2
        own = list(range(32 * bq + 16 * (cs % 2), 32 * bq + 16 * (cs % 2) + 16))
        rest = [t for t in range(32 * bq, 32 * bq + 32) if t not in own]
        other = list(range(32 * (1 - bq), 32 * (1 - bq) + 32))
        order = own + rest + other
        orders.append(order)
        cfgs.append((xs, order, True, bq, inp["mem_sample"][bq], inp["mem_sample"][1 - bq]))
    key = (NT, OWN, NE, CAP)
    if key not in _NC_CACHE:
        _NC_CACHE[key] = build(NT, OWN, NE, CAP)
    nc = _NC_CACHE[key]
    maps = _core_inputs(inp, NT, OWN, CAP, cfgs)
    res = run_bass_kernel_spmd(nc, maps, core_ids=list(range(8)))
    yp = np.zeros((1, 8192, D), np.float32)
    ys = np.zeros((2, 4096, D), np.float32)
    ysf = ys.reshape(-1, D)
    for c in range(8):
        y = np.asarray(res.results[c]["y"], dtype=np.float32)
        own = orders[c][:OWN]
        dst = yp[0] if c < 4 else ysf
        for i, t in enumerate(own):
            dst[t * 128:(t + 1) * 128] = y[i * 128:(i + 1) * 128]
    return (yp, ys)
```

```python
import contextlib
import math
import numpy as np
import concourse.bass as bass
import concourse.mybir as mybir
from concourse.bass_utils import run_bass_kernel_spmd

F32 = mybir.dt.float32
BF16 = mybir.dt.bfloat16
ALU = mybir.AluOpType
AF = mybir.ActivationFunctionType
AX = mybir.AxisListType

D = 2048
KD = 16
DIN = 3072
DG = 1024
NH = 8
NM = 256
DE = 4096
FC = 32
EPS = 1e-6
LN_EPS = 1e-5


class Bld:
    NPOOL = 24

    def __init__(self, nc):
        self.nc = nc
        self.st = contextlib.ExitStack()
        self.esem = {e: self.st.enter_context(nc.semaphore("s_" + e)) for e in ("pe", "act", "dve", "pool")}
        self.dsem = {q: [self.st.enter_context(nc.semaphore("d_%s_%d" % (q, k))) for k in range(self.NPOOL)]
                     for q in ("sp", "pool", "act")}
        self.cnt = {e: 0 for e in self.esem}
        self.dcnt = {q: 0 for q in self.dsem}
        self.duse = {q: [0] * self.NPOOL for q in self.dsem}
        self.waited = {e: {} for e in ("pe", "act", "dve", "pool", "sp")}
        self.reset()
        self.nseg = 0

    def reset(self):
        self.ops = []
        self.lastw = {}
        self.readers = {}

    def op(self, eng, fn, r=(), w=(), dma=False):
        i = len(self.ops)
        deps = set()
        for k in r:
            if k in self.lastw:
                deps.add(self.lastw[k])
        for k in w:
            if k in self.lastw:
                deps.add(self.lastw[k])
            deps.update(self.readers.get(k, ()))
        for k in r:
            self.readers.setdefault(k, []).append(i)
        for k in w:
            self.lastw[k] = i
            self.readers[k] = []
        self.ops.append(dict(eng=eng, fn=fn, deps=deps, dma=dma))
        return i

    def dma(self, q, out, in_, r=(), w=(), **kw):
        return self.op(q, lambda e: e.dma_start(out=out, in_=in_, **kw), r, w, dma=True)

    def flush(self, last=False):
        nc = self.nc
        ops = self.ops
        needed = [False] * len(ops)
        for o in ops:
            for d in o["deps"]:
                needed[d] = True
        barrier = [(("e", e), self.esem[e], self.cnt[e]) for e in self.esem]
        barrier += [(("d", q, k), self.dsem[q][k], 16 * self.duse[q][k])
                    for q in self.dsem for k in range(self.NPOOL)]
        last_on = {}
        for i, o in enumerate(ops):
            last_on[o["eng"]] = i
        for e, i in last_on.items():
            needed[i] = True
        for i, o in enumerate(ops):
            if o["dma"]:
                q = o["eng"]
                k = self.dcnt[q] % self.NPOOL
                self.dcnt[q] += 1
                o["prev"] = (self.dsem[q][k], 16 * self.duse[q][k])
                self.duse[q][k] += 1
                o["done"] = (self.dsem[q][k], 16 * self.duse[q][k])
                o["key"] = ("d", q, k)
            elif needed[i]:
                self.cnt[o["eng"]] += 1
                o["done"] = (self.esem[o["eng"]], self.cnt[o["eng"]])
                o["key"] = ("e", o["eng"])
            else:
                o["done"] = None
        final = [(("d", q, k), self.dsem[q][k], 16 * self.duse[q][k])
                 for q in self.dsem for k in range(self.NPOOL)]
        first_seg = self.nseg == 0
        self.nseg += 1

        def run(engname, eng):
            waited = self.waited[engname]

            def wait(sem, val, key):
                if val <= 0 or waited.get(key, 0) >= val:
                    return
                eng.wait_ge(sem, val)
                waited[key] = val

            if not first_seg:
                for key, sem, val in barrier:
                    if key == ("e", engname):
                        continue
                    wait(sem, val, key)
            for i, o in enumerate(ops):
                if o["eng"] != engname:
                    continue
                for d in sorted(o["deps"]):
                    od = ops[d]
                    if od["eng"] == "pe" and engname == "pe" and not od["dma"]:
                        continue
                    sem, val = od["done"]
                    wait(sem, val, od["key"])
                if o["dma"]:
                    wait(o["prev"][0], o["prev"][1], o["key"])
                ins = o["fn"](eng)
                if o["done"] is not None:
                    ins.then_inc(o["done"][0], 16 if o["dma"] else 1)
            if last and engname == "sp":
                for key, sem, val in final:
                    wait(sem, val, key)

        with nc.Block() as block:
            @block.sync
            def _(e):
                run("sp", e)

            @block.gpsimd
            def _(e):
                run("pool", e)

            @block.scalar
            def _(e):
                run("act", e)

            @block.vector
            def _(e):
                run("dve", e)

            @block.tensor
            def _(e):
                run("pe", e)
        self.reset()

    def close(self):
        self.st.close()


def build(NT, OWN, NE, CAP, debug=False):
    JH = NT // 2
    NCC = CAP // 128
    KSEL = float(2 * NT * 128 // NE)
    nc = bass.Bass("TRN2", target_bir_lowering=False)

    def IN(name, shape, dt=F32):
        return nc.dram_tensor(name, shape, dt, kind="ExternalInput").ap()

    def SCR(name, shape, dt):
        return nc.dram_tensor(name, shape, dt).ap()

    xg = IN("xg", [NT * 128, D])
    memA = IN("memA", [NM, D])
    memB = IN("memB", [NM, D])
    w_in = IN("w_in", [D, DIN])
    w_spT = IN("w_spT", [NH, 128, 128])
    bsp_i = IN("bsp", [128, NH])
    lng_i = IN("lng", [128, DG])
    lnb_i = IN("lnb", [128, DG])
    wf_i = IN("w_fourier", [NH, 128, 128])
    goa_i = IN("goa", [128, DG])
    gob_i = IN("gob", [128, DG])
    w_out = IN("w_out", [D, D])
    gmix_i = IN("gmix", [128, D])
    gattn_i = IN("gattn", [128, D])
    gmem_i = IN("gmem", [128, D])
    gffn_i = IN("gffn", [128, D])
    gfin_i = IN("gfin", [128, D])
    w_q = IN("w_q", [D, D])
    w_kv = IN("w_kv", [D, 2 * D])
    w_o = IN("w_o", [D, D])
    w_r = IN("w_router", [D, NE])
    br_i = IN("br", [128, NE])
    w_gate = IN("w_gate", [NE, D, DE])
    w_up = IN("w_up", [NE, D, DE])
    w_down = IN("w_down", [NE, DE, D])
    cc_i = IN("cc", [128, 128])
    sc_i = IN("sc", [128, 128])
    c1_i = IN("c1", [NT, 2, 3, 128])
    m2re_i = IN("m2re", [128, 2 * 128 * JH])
    m2im_i = IN("m2im", [128, 2 * 128 * JH])
    ident_i = IN("ident", [128, 128])
    ones_i = IN("ones", [128, 128])
    tri_i = IN("tri", [128, 128])
    iota_i = IN("iota", [128, CAP])
    y_out = nc.dram_tensor("y", [OWN * 128, D], F32, kind="ExternalOutput").ap()
    dbg = {}
    if debug:
        for nm, shp in [("d_h1", [NT * 128, D]), ("d_h2", [OWN * 128, D]), ("d_aff", [NT * 128, NE]),
                        ("d_yb", [NT * 128, DG]), ("d_thr", [128, NE])]:
            dbg[nm] = nc.dram_tensor(nm, shp, F32, kind="ExternalOutput").ap()

    ya_d = SCR("ya_d", [NT * 128, DG], BF16)
    ab_d = SCR("ab_d", [NT * 128, 2 * DG], BF16)
    g_d = SCR("g_d", [2, 128, 128, 2, DG], BF16)
    yb_d = dbg["d_yb"] if debug else SCR("yb_d", [NT * 128, DG], F32)
    h1_d = dbg["d_h1"] if debug else SCR("h1_d", [NT * 128, D], F32)
    o_d = SCR("o_d", [NT * 128, D], BF16)
    h2_d = dbg["d_h2"] if debug else SCR("h2_d", [OWN * 128, D], F32)
    aff_d = dbg["d_aff"] if debug else SCR("aff_d", [NT * 128, NE], F32)
    xn3_d = SCR("xn3_d", [OWN * 128, D], BF16)
    yex_d = SCR("yex_d", [NE, CAP, D], BF16)

    top = contextlib.ExitStack()
    b = Bld(nc)

    uid = [0]

    def SBt(st, name, shape, dt=F32):
        uid[0] += 1
        return st.enter_context(nc.sbuf_tensor("%s_%d" % (name, uid[0]), shape, dt))

    ptr = [top.enter_context(nc.psum_tensor("ptr%d" % i, [128, 1024], BF16)) for i in range(2)]
    pf = [top.enter_context(nc.psum_tensor("pf%d" % i, [128, 512], F32)) for i in range(6)]
    PTR = ["ptr0", "ptr1"]
    PF = ["pf%d" % i for i in range(6)]
    ident = SBt(top, "ident", [128, 128], BF16)
    identf = SBt(top, "identf", [128, 128], F32)
    ones = SBt(top, "ones", [128, 128], BF16)
    tri = SBt(top, "tri", [128, 128], BF16)
    iota = SBt(top, "iota", [128, CAP], F32)
    ss = SBt(top, "ss", [128, 16], F32)
    b.dma("pool", ident[:], ident_i, w=["ident"])
    b.dma("sp", identf[:], ident_i, w=["identf"])
    b.dma("pool", ones[:], ones_i, w=["ones"])
    b.dma("pool", tri[:], tri_i, w=["tri"])
    b.dma("sp", iota[:], iota_i, w=["iota"])

    cpy_rr = [0]

    def evac(out, in_, r, w, scale=None):
        cpy_rr[0] += 1
        if scale is not None or cpy_rr[0] % 2 == 0:
            if scale is None:
                b.op("act", lambda e: e.copy(out=out, in_=in_), r=r, w=w)
            else:
                b.op("act", lambda e: e.activation(out=out, in_=in_, func=AF.Copy, scale=scale), r=r, w=w)
        else:
            b.op("dve", lambda e: e.tensor_copy(out=out, in_=in_), r=r, w=w)

    def rstd_of(src, n, eps, rkey, col, skey, scr, scrkey):
        sc = ss[:, col:col + 1]
        b.op("act", lambda e: e.activation(out=scr, in_=src, func=AF.Square, accum_out=sc),
             r=[rkey], w=[scrkey, skey])
        b.op("act", lambda e: e.activation(out=sc, in_=sc, func=AF.Sqrt, bias=float(eps), scale=1.0 / n),
             r=[skey], w=[skey])
        b.op("dve", lambda e: e.reciprocal(out=sc, in_=sc), r=[skey], w=[skey])
        return sc

    def transposes(src, skey, dst, dkey, n, idt=None, fp32=False, fbase=0):
        if not fp32:
            for g0 in range(0, n, 8):
                bank = (g0 // 8) % 2
                gn = min(8, n - g0)
                for i in range(gn):
                    b.op("pe", lambda e, i=i, g0=g0, bank=bank: e.transpose(
                        ptr[bank][:, i * 128:(i + 1) * 128], src[:, (g0 + i) * 128:(g0 + i + 1) * 128], ident[:]),
                        r=[skey, "ident"], w=[PTR[bank]])
                evac(dst[:, g0:g0 + gn, :], ptr[bank][:, 0:gn * 128].rearrange("p (a c) -> p a c", a=gn),
                     r=[PTR[bank]], w=[dkey])
        else:
            for g0 in range(0, n, 4):
                bank = fbase + (g0 // 4) % 2
                for i in range(4):
                    b.op("pe", lambda e, i=i, g0=g0, bank=bank: e.transpose(
                        pf[bank][:, i * 128:(i + 1) * 128], src[:, (g0 + i) * 128:(g0 + i + 1) * 128], identf[:]),
                        r=[skey, "identf"], w=[PF[bank]])
                evac(dst[:, g0:g0 + 4, :], pf[bank][:, :].rearrange("p (a c) -> p a c", a=4),
                     r=[PF[bank]], w=[dkey])

    def load_w(st, name, src, ncols, q="pool"):
        t = SBt(st, name, [128, KD, ncols], BF16)
        v = src.rearrange("(kc p) n -> p kc n", p=128)
        for kc in range(KD):
            b.dma(q, t[:, kc, :], v[:, kc, :], w=[name])
        return t

    def proj(xT, xkey, w_sb, wkey, ncols, sink, base=0):
        for ch in range(ncols // 512):
            k = base + ch % 2
            for kc in range(KD):
                b.op("pe", lambda e, k=k, kc=kc, ch=ch: e.matmul(
                    pf[k][:, :], lhsT=xT[:, kc, :], rhs=w_sb[:, kc, ch * 512:(ch + 1) * 512],
                    start=(kc == 0), stop=(kc == KD - 1)), r=[xkey, wkey], w=[PF[k]])
            sink(ch, pf[k], PF[k])

    st = contextlib.ExitStack()
    st0 = contextlib.ExitStack()
    cswf = SBt(st, "cswf", [128, NH, 256], BF16)
    wf = SBt(st0, "wf", [128, NH, 128], F32)
    ccf = SBt(st0, "ccf", [128, 128], F32)
    scf = SBt(st0, "scf", [128, 128], F32)
    b.dma("sp", wf[:], wf_i.rearrange("g c e -> c g e"), w=["wf"])
    b.dma("sp", ccf[:], cc_i, w=["ccf"])
    b.dma("sp", scf[:], sc_i, w=["scf"])
    for ci, (cm, ck) in enumerate([(ccf, "ccf"), (scf, "scf")]):
        for hf in range(2):
            k = ci * 2 + hf
            b.op("pe", lambda e, cm=cm, hf=hf, k=k: e.matmul(
                pf[k][:, :], lhsT=cm[:], rhs=wf[:].rearrange("p g e -> p (g e)")[:, hf * 512:(hf + 1) * 512], start=True, stop=True),
                r=[ck, "wf"], w=[PF[k]])
            evac(cswf[:, hf * 4:(hf + 1) * 4, ci * 128:(ci + 1) * 128],
                 pf[k][:, :].rearrange("p (g e) -> p g e", g=4), r=[PF[k]], w=["cswf"])
    b.flush()
    st0.close()
    win = load_w(st, "win", w_in, DIN)
    wsT = SBt(st, "wsT", [128, NH, 128], BF16)
    b.dma("pool", wsT[:], w_spT.rearrange("h q p -> q h p"), w=["wsT"])
    gmix = SBt(st, "gmix", [128, D]); b.dma("sp", gmix[:], gmix_i, w=["gmix"])
    lng = SBt(st, "lng", [128, DG]); b.dma("sp", lng[:], lng_i, w=["lng"])
    lnb = SBt(st, "lnb", [128, DG]); b.dma("sp", lnb[:], lnb_i, w=["lnb"])
    goa = SBt(st, "goa", [128, DG]); b.dma("sp", goa[:], goa_i, w=["goa"])
    bsp = SBt(st, "bspt", [128, NH]); b.dma("sp", bsp[:], bsp_i, w=["bsp"])
    xt = [SBt(st, "xt%d" % i, [128, D]) for i in range(2)]
    xn = [SBt(st, "xn%d" % i, [128, D], BF16) for i in range(2)]
    xnT = [SBt(st, "xnT%d" % i, [128, KD, 128], BF16) for i in range(2)]
    u = [SBt(st, "u%d" % i, [128, DG], BF16) for i in range(2)]
    v = [SBt(st, "v%d" % i, [128, DG], BF16) for i in range(2)]
    pb = [SBt(st, "pb%d" % i, [128, DG], BF16) for i in range(2)]
    t1 = SBt(st, "t1", [128, DG])
    vn = SBt(st, "vn", [128, DG], BF16)
    gt = t1
    ya0 = SBt(st, "ya0", [128, DG], BF16)
    pbT = SBt(st, "pbT", [128, NH, 128], BF16)
    ab0 = SBt(st, "ab0", [128, 2 * DG], BF16)
    st8 = SBt(st, "st8", [128, 4, NH])

    def v3(t):
        return t[:].rearrange("p (h d) -> p h d", h=NH)

    def b8(col):
        return st8[:, col, :, None].broadcast_to([128, NH, 128])

    def p1_S0(T):
        i2 = T % 2
        X = xt[i2]; xk = "xt%d" % i2
        XN = xn[i2]; xnk = "xn%d" % i2
        b.dma("sp", X[:], xg[T * 128:(T + 1) * 128, :], w=[xk])
        rs = rstd_of(X[:], D, EPS, xk, i2, "ssx%d" % i2, XN[:], xnk)
        b.op("dve", lambda e: e.scalar_tensor_tensor(out=XN[:], in0=X[:], scalar=rs, in1=gmix[:],
                                                     op0=ALU.mult, op1=ALU.mult),
             r=[xk, "ssx%d" % i2, "gmix"], w=[xnk])

    def p1_S1(T):
        i2 = T % 2
        transposes(xn[i2], "xn%d" % i2, xnT[i2], "xnT%d" % i2, KD)

    def p1_S2(T):
        i2 = T % 2
        U = u[i2]; V = v[i2]; PB = pb[i2]
        uk = "u%d" % i2; vk = "v%d" % i2; pbk = "pb%d" % i2

        def sink1(ch, ps, pk):
            if ch < 2:
                b.op("act", lambda e: e.activation(out=U[:, ch * 512:(ch + 1) * 512], in_=ps[:, :],
                                                   func=AF.Gelu_apprx_tanh), r=[pk], w=[uk])
            elif ch < 4:
                b.op("act", lambda e: e.activation(out=V[:, (ch - 2) * 512:(ch - 1) * 512], in_=ps[:, :],
                                                   func=AF.Gelu_apprx_tanh), r=[pk], w=[vk])
            else:
                b.op("act", lambda e: e.copy(out=PB[:, (ch - 4) * 512:(ch - 3) * 512], in_=ps[:, :]),
                     r=[pk], w=[pbk])
        proj(xnT[i2], "xnT%d" % i2, win, "win", DIN, sink1)

    def p1_S3a(T):
        i2 = T % 2
        V = v[i2]; vk = "v%d" % i2
        b.op("dve", lambda e: e.tensor_reduce(out=st8[:, 0, :], in_=v3(V), axis=AX.X, op=ALU.add), r=[vk], w=["st8a"])
        b.op("act", lambda e: e.activation(out=t1[:], in_=V[:], func=AF.Square), r=[vk], w=["t1"])
        b.op("dve", lambda e: e.tensor_reduce(out=st8[:, 1, :], in_=v3(t1), axis=AX.X, op=ALU.add), r=["t1"], w=["st8b"])
        b.op("dve", lambda e: e.tensor_scalar(out=st8[:, 0, :], in0=st8[:, 0, :], scalar1=1.0 / 128, scalar2=None,
                                              op0=ALU.mult), r=["st8a"], w=["st8a"])
        b.op("dve", lambda e: e.tensor_tensor(out=st8[:, 2, :], in0=st8[:, 0, :], in1=st8[:, 0, :], op=ALU.mult),
             r=["st8a"], w=["st8c"])
        b.op("dve", lambda e: e.scalar_tensor_tensor(out=st8[:, 2, :], in0=st8[:, 1, :], scalar=1.0 / 128,
                                                     in1=st8[:, 2, :], op0=ALU.mult, op1=ALU.subtract),
             r=["st8b", "st8c"], w=["st8c"])
        b.op("act", lambda e: e.activation(out=st8[:, 3, :], in_=st8[:, 2, :], func=AF.Sqrt, bias=float(LN_EPS),
                                           scale=1.0), r=["st8c"], w=["st8d"])
        b.op("dve", lambda e: e.reciprocal(out=st8[:, 3, :], in_=st8[:, 3, :]), r=["st8d"], w=["st8d"])
        b.op("dve", lambda e: e.tensor_tensor(out=v3(t1), in0=v3(V), in1=b8(0), op=ALU.subtract),
             r=[vk, "st8a"], w=["t1"])
        b.op("dve", lambda e: e.tensor_tensor(out=v3(t1), in0=v3(t1), in1=b8(3), op=ALU.mult),
             r=["t1", "st8d"], w=["t1"])
        b.op("dve", lambda e: e.tensor_tensor(out=t1[:], in0=t1[:], in1=lng[:], op=ALU.mult), r=["t1", "lng"], w=["t1"])
        b.op("dve", lambda e: e.tensor_tensor(out=vn[:], in0=t1[:], in1=lnb[:], op=ALU.add), r=["t1", "lnb"], w=["vn"])

    def p1_S3b(T):
        i2 = T % 2
        U = u[i2]; PB = pb[i2]
        uk = "u%d" % i2; pbk = "pb%d" % i2
        transposes(PB, pbk, pbT, "pbT", NH)
        for hf in range(2):
            for gl in range(4):
                g = hf * 4 + gl
                k = 4 + gl // 2
                b.op("pe", lambda e, g=g, gl=gl, k=k: e.matmul(pf[k][:, (gl % 2) * 256:(gl % 2 + 1) * 256],
                                                               lhsT=pbT[:, g, :], rhs=cswf[:, g, :], start=True, stop=True),
                     r=["pbT", "cswf"], w=[PF[k]])
                if gl % 2 == 1:
                    g0 = g - 1
                    evac(ab0[:].rearrange("p (r g e) -> p r g e", r=2, g=NH)[:, :, g0:g0 + 2, :],
                         pf[k][:, :].rearrange("p (g r e) -> p r g e", g=2, r=2), r=[PF[k]], w=["ab0"])
        b.dma("pool", ab_d[T * 128:(T + 1) * 128, :], ab0[:], r=["ab0"], w=[("ab_d", T)])
        for h in range(NH):
            k = 2 + h // 4
            b.op("pe", lambda e, h=h, k=k: e.matmul(pf[k][:, (h % 4) * 128:(h % 4 + 1) * 128], lhsT=wsT[:, h, :],
                                                    rhs=vn[:, h * 128:(h + 1) * 128], start=True, stop=True),
                 r=["wsT", "vn"], w=[PF[k]])
        for hf in range(2):
            b.op("dve", lambda e, hf=hf: e.tensor_tensor(
                out=gt[:, hf * 512:(hf + 1) * 512].rearrange("p (h d) -> p h d", h=4),
                in0=pf[2 + hf][:, :].rearrange("p (h d) -> p h d", h=4),
                in1=bsp[:, hf * 4:(hf + 1) * 4, None].broadcast_to([128, 4, 128]), op=ALU.add),
                r=[PF[2 + hf], "bsp"], w=["t1"])
        b.op("dve", lambda e: e.tensor_tensor(out=gt[:], in0=gt[:], in1=U[:], op=ALU.mult), r=["t1", uk], w=["t1"])
        rs2 = rstd_of(gt[:], DG, EPS, "t1", 2, "ss2", vn[:], "vn")
        b.op("dve", lambda e: e.scalar_tensor_tensor(out=ya0[:], in0=gt[:], scalar=rs2, in1=goa[:],
                                                     op0=ALU.mult, op1=ALU.mult),
             r=["t1", "ss2", "goa"], w=["ya0"])
        b.dma("pool", ya_d[T * 128:(T + 1) * 128, :], ya0[:], r=["ya0"], w=[("ya_d", T)])

    p1_S0(0)
    for i in range(NT + 2):
        if i + 1 < NT:
            p1_S0(i + 1)
        if i < NT:
            p1_S1(i)
        if 2 <= i:
            p1_S3a(i - 2)
        if 1 <= i <= NT:
            p1_S2(i - 1)
        if 2 <= i:
            p1_S3b(i - 2)
    b.flush()
    st.close()

    st = contextlib.ExitStack()
    FB = 4
    c1 = SBt(st, "c1", [NT, 2, 3, 128], BF16)
    b.dma("pool", c1[:], c1_i, w=["c1"])
    dat = [SBt(st, "dat%d" % i, [NT, FB, 2 * DG], BF16) for i in range(2)]
    gs = [SBt(st, "gs%d" % i, [128, 2, FB, 2, DG], BF16) for i in range(2)]
    abv = ab_d.rearrange("(p f) e -> p f e", f=128)
    rr = 0
    for fb in range(128 // FB):
        DT = dat[fb % 2]; dk = "dat%d" % (fb % 2)
        GS = gs[fb % 2]; gk = "gs%d" % (fb % 2)
        b.dma("sp", DT[:], abv[:, fb * FB:(fb + 1) * FB, :], w=[dk])
        for fl in range(FB):
            for m in range(2):
                for eh in range(2):
                    for ri in range(2):
                        k = rr % 6; rr += 1
                        l0, l1 = (0, 1) if ri == 0 else (1, 2)
                        b.op("pe", lambda e, k=k, m=m, l0=l0, fl=fl, eh=eh, DT=DT: e.matmul(
                            pf[k][:, :], lhsT=c1[:, m, l0, :], rhs=DT[:, fl, eh * 512:(eh + 1) * 512],
                            start=True, stop=False), r=["c1", dk], w=[PF[k]])
                        b.op("pe", lambda e, k=k, m=m, l1=l1, fl=fl, eh=eh, DT=DT: e.matmul(
                            pf[k][:, :], lhsT=c1[:, m, l1, :], rhs=DT[:, fl, DG + eh * 512:DG + (eh + 1) * 512],
                            start=False, stop=True), r=["c1", dk], w=[PF[k]])
                        evac(GS[:, m, fl, ri, eh * 512:(eh + 1) * 512], pf[k][:, :], r=[PF[k]], w=[gk])
        for m in range(2):
            b.dma("pool", g_d[m, :, fb * FB:(fb + 1) * FB, :, :], GS[:, m, :, :, :], r=[gk], w=[("g_d", m, fb)])
    b.flush()
    st.close()

    stW = contextlib.ExitStack()
    wout = load_w(stW, "wout", w_out, D)
    st = contextlib.ExitStack()
    QB = 4
    m2re = SBt(st, "m2re", [128, 2, 128, JH], BF16)
    m2im = SBt(st, "m2im", [128, 2, 128, JH], BF16)
    b.dma("pool", m2re[:].rearrange("p m q j -> p (m q j)"), m2re_i, w=["m2re"])
    b.dma("pool", m2im[:].rearrange("p m q j -> p (m q j)"), m2im_i, w=["m2im"])
    gin = [SBt(st, "gin%d" % i, [128, QB, 2, DG], BF16) for i in range(2)]
    yst = [SBt(st, "yst%d" % i, [JH, QB, DG], F32) for i in range(2)]
    ybv = yb_d.rearrange("(m j q) e -> m j q e", m=2, j=JH)
    it = 0
    for m in range(2):
        for qb in range(128 // QB):
            GI = gin[it % 2]; gik = "gin%d" % (it % 2)
            YS = yst[it % 2]; ysk = "yst%d" % (it % 2)
            it += 1
            b.dma("sp", GI[:], g_d[m, qb * QB:(qb + 1) * QB, :, :, :].rearrange("q f r e -> f q r e"),
                  r=[("g_d", m, fb) for fb in range(128 // FB)], w=[gik])
            for ql in range(QB):
                q = qb * QB + ql
                for eh in range(2):
                    k = rr % 6; rr += 1
                    b.op("pe", lambda e, k=k, m=m, q=q, ql=ql, eh=eh, GI=GI: e.matmul(
                        pf[k][0:JH, :], lhsT=m2re[:, m, q, :], rhs=GI[:, ql, 0, eh * 512:(eh + 1) * 512],
                        start=True, stop=False), r=["m2re", gik], w=[PF[k]])
                    b.op("pe", lambda e, k=k, m=m, q=q, ql=ql, eh=eh, GI=GI: e.matmul(
                        pf[k][0:JH, :], lhsT=m2im[:, m, q, :], rhs=GI[:, ql, 1, eh * 512:(eh + 1) * 512],
                        start=False, stop=True), r=["m2im", gik], w=[PF[k]])
                    evac(YS[:, ql, eh * 512:(eh + 1) * 512], pf[k][0:JH, :], r=[PF[k]], w=[ysk])
            b.dma("pool", ybv[m, :, qb * QB:(qb + 1) * QB, :], YS[:], r=[ysk], w=[("yb_d", m, qb)])
    b.flush()
    st.close()

    st = contextlib.ExitStack()
    gob = SBt(st, "gob", [128, DG]); b.dma("sp", gob[:], gob_i, w=["gob"])
    xt = [SBt(st, "xt%d" % i, [128, D]) for i in range(4)]
    ybt = [SBt(st, "ybt%d" % i, [128, DG]) for i in range(2)]
    cat = [SBt(st, "cat%d" % i, [128, D], BF16) for i in range(2)]
    catT = [SBt(st, "catT%d" % i, [128, KD, 128], BF16) for i in range(2)]
    h1 = [SBt(st, "h1_%d" % i, [128, D]) for i in range(2)]

    def a_S0(T):
        i2 = T % 2; i3 = T % 4
        rows = slice(T * 128, (T + 1) * 128)
        b.dma("sp", ybt[i2][:], yb_d[rows, :], w=["ybt%d" % i2])
        b.dma("sp", cat[i2][:, 0:DG], ya_d[rows, :], w=["cat%d" % i2])
        b.dma("sp", xt[i3][:], xg[rows, :], w=["xt%d" % i3])
        rs = rstd_of(ybt[i2][:], DG, EPS, "ybt%d" % i2, i2, "ssy%d" % i2, cat[i2][:, DG:D], "cat%d" % i2)
        b.op("dve", lambda e: e.scalar_tensor_tensor(out=cat[i2][:, DG:D], in0=ybt[i2][:], scalar=rs,
                                                     in1=gob[:], op0=ALU.mult, op1=ALU.mult),
             r=["ybt%d" % i2, "ssy%d" % i2, "gob"], w=["cat%d" % i2])

    def a_S1(T):
        i2 = T % 2
        transposes(cat[i2], "cat%d" % i2, catT[i2], "catT%d" % i2, KD)

    def a_S2(T):
        i2 = T % 2; i3 = T % 4
        rows = slice(T * 128, (T + 1) * 128)

        def sink2(ch, ps, pk):
            b.op("dve", lambda e: e.tensor_tensor(out=h1[i2][:, ch * 512:(ch + 1) * 512], in0=ps[:, :],
                                                  in1=xt[i3][:, ch * 512:(ch + 1) * 512], op=ALU.add),
                 r=[pk, "xt%d" % i3], w=["h1_%d" % i2])
        proj(catT[i2], "catT%d" % i2, wout, "wout", D, sink2)
        b.dma("pool", h1_d[rows, :], h1[i2][:], r=["h1_%d" % i2], w=[("h1_d", T)])

    a_S0(0)
    for i in range(NT + 1):
        if i < NT:
            a_S1(i)
        if i + 1 < NT:
            a_S0(i + 1)
        if i >= 1:
            a_S2(i - 1)
    b.flush()
    st.close()
    stW.close()

    st = contextlib.ExitStack()
    st0 = contextlib.ExitStack()
    kT = SBt(st, "kT", [128, 2, KD, NM], BF16)
    vsb = SBt(st, "vsb", [128, 2, 2, D], BF16)
    wq = load_w(st, "wq", w_q, D)
    gmem = SBt(st0, "gmem", [128, D]); b.dma("sp", gmem[:], gmem_i, w=["gmem"])
    memT = SBt(st0, "memT", [128, 2, KD, NM], BF16)
    mt = SBt(st0, "mt", [128, D])
    mn = SBt(st0, "mn", [128, D], BF16)
    mT1 = SBt(st0, "mT1", [128, KD, 128], BF16)
    wkv = [SBt(st0, "wkv%d" % i, [128, KD, 512], BF16) for i in range(2)]
    for a, msrc in enumerate([memA, memB]):
        for c in range(2):
            b.dma("sp", mt[:], msrc[c * 128:(c + 1) * 128, :], w=["mt"])
            rs = rstd_of(mt[:], D, EPS, "mt", 0, "ss0", mn[:], "mn")
            b.op("dve", lambda e, rs=rs: e.scalar_tensor_tensor(out=mn[:], in0=mt[:], scalar=rs, in1=gmem[:],
                                                                op0=ALU.mult, op1=ALU.mult),
                 r=["mt", "ss0", "gmem"], w=["mn"])
            transposes(mn, "mn", mT1, "mT1", KD)
            b.op("dve", lambda e, a=a, c=c: e.tensor_copy(out=memT[:, a, :, c * 128:(c + 1) * 128], in_=mT1[:]),
                 r=["mT1"], w=["memT"])
    wkvv = w_kv.rearrange("(kc p) n -> p kc n", p=128)
    for ch in range(8):
        WK = wkv[ch % 2]; wk = "wkv%d" % (ch % 2)
        b.dma("pool", WK[:], wkvv[:, :, ch * 512:(ch + 1) * 512], w=[wk])
        for a in range(2):
            if ch < 4:
                for dl in range(4):
                    dc = ch * 4 + dl
                    k = rr % 6; rr += 1
                    for kc in range(KD):
                        b.op("pe", lambda e, k=k, kc=kc, dl=dl, a=a, WK=WK: e.matmul(
                            pf[k][:, 0:NM], lhsT=WK[:, kc, dl * 128:(dl + 1) * 128], rhs=memT[:, a, kc, :],
                            start=(kc == 0), stop=(kc == KD - 1)), r=[wk, "memT"], w=[PF[k]])
                    evac(kT[:, a, dc, :], pf[k][:, 0:NM], r=[PF[k]], w=["kT"])
            else:
                for c in range(2):
                    k = rr % 6; rr += 1
                    for kc in range(KD):
                        b.op("pe", lambda e, k=k, kc=kc, c=c, a=a, WK=WK: e.matmul(
                            pf[k][:, :], lhsT=memT[:, a, kc, c * 128:(c + 1) * 128], rhs=WK[:, kc, :],
                            start=(kc == 0), stop=(kc == KD - 1)), r=[wk, "memT"], w=[PF[k]])
                    evac(vsb[:, a, c, (ch - 4) * 512:(ch - 3) * 512], pf[k][:, :], r=[PF[k]], w=["vsb"])
    b.flush()
    st0.close()
    gattn = SBt(st, "gattn", [128, D]); b.dma("sp", gattn[:], gattn_i, w=["gattn"])
    h1t = [SBt(st, "h1t%d" % i, [128, D]) for i in range(2)]
    xn2 = [SBt(st, "xn2_%d" % i, [128, D], BF16) for i in range(2)]
    xn2T = [SBt(st, "xn2T%d" % i, [128, KD, 128], BF16) for i in range(2)]
    qs = [SBt(st, "qs%d" % i, [128, D], BF16) for i in range(2)]
    qT = SBt(st, "qT", [128, KD, 128], BF16)
    pr = [SBt(st, "pr%d" % i, [128, 4 * NM], BF16) for i in range(2)]
    prT = SBt(st, "prT", [128, 8, 128], BF16)
    osb = [SBt(st, "osb%d" % i, [128, D], BF16) for i in range(2)]
    sm = [SBt(st, "sm%d" % i, [128, 3, 4]) for i in range(2)]

    def b_S0(T):
        i2 = T % 2
        rows = slice(T * 128, (T + 1) * 128)
        H = h1t[i2]; hk = "h1t%d" % i2
        XN = xn2[i2]; xk = "xn2_%d" % i2
        b.dma("sp", H[:], h1_d[rows, :], w=[hk])
        rs = rstd_of(H[:], D, EPS, hk, i2, "ssh%d" % i2, XN[:], xk)
        b.op("dve", lambda e: e.scalar_tensor_tensor(out=XN[:], in0=H[:], scalar=rs, in1=gattn[:],
                                                     op0=ALU.mult, op1=ALU.mult),
             r=[hk, "ssh%d" % i2, "gattn"], w=[xk])

    def b_S1(T):
        i2 = T % 2
        transposes(xn2[i2], "xn2_%d" % i2, xn2T[i2], "xn2T%d" % i2, KD)

    def b_S2(T):
        i2 = T % 2
        Q = qs[i2]; qk = "qs%d" % i2

        def sink3(ch, ps, pk):
            b.op("act", lambda e: e.activation(out=Q[:, ch * 512:(ch + 1) * 512], in_=ps[:, :], func=AF.Copy,
                                               scale=float(512 ** -0.5)), r=[pk], w=[qk])
        proj(xn2T[i2], "xn2T%d" % i2, wq, "wq", D, sink3)

    def b_S3a(T):
        i2 = T % 2
        transposes(qs[i2], "qs%d" % i2, qT, "qT", KD)

    def b_S3b(T):
        i2 = T % 2
        a = 0 if T < NT // 2 else 1
        SM = sm[i2]; PR = pr[i2]
        s0 = "sm0_%d" % i2; s1 = "sm1_%d" % i2; s2 = "sm2_%d" % i2; prk = "pr%d" % i2
        for h in range(4):
            k = 2 + h // 2
            for c in range(4):
                b.op("pe", lambda e, h=h, c=c, k=k: e.matmul(
                    pf[k][:, (h % 2) * NM:(h % 2 + 1) * NM], lhsT=qT[:, 4 * h + c, :], rhs=kT[:, a, 4 * h + c, :],
                    start=(c == 0), stop=(c == 3)), r=["qT", "kT"], w=[PF[k]])
        for hp in range(2):
            b.op("dve", lambda e, hp=hp: e.tensor_reduce(out=SM[:, 0, hp * 2:hp * 2 + 2],
                                                         in_=pf[2 + hp][:, :].rearrange("p (h m) -> p h m", h=2),
                                                         axis=AX.X, op=ALU.max), r=[PF[2 + hp]], w=[s0])
        b.op("dve", lambda e: e.tensor_scalar(out=SM[:, 0, :], in0=SM[:, 0, :], scalar1=-1.0, scalar2=None,
                                              op0=ALU.mult), r=[s0], w=[s0])
        for h in range(4):
            b.op("act", lambda e, h=h: e.activation(out=PR[:, h * NM:(h + 1) * NM],
                                                    in_=pf[2 + h // 2][:, (h % 2) * NM:(h % 2 + 1) * NM], func=AF.Exp,
                                                    bias=SM[:, 0, h:h + 1], scale=1.0, accum_out=SM[:, 1, h:h + 1]),
                 r=[PF[2 + h // 2], s0], w=[prk, s1])
        b.op("dve", lambda e: e.reciprocal(out=SM[:, 2, :], in_=SM[:, 1, :]), r=[s1], w=[s2])

    def b_S4(T):
        i2 = T % 2
        a = 0 if T < NT // 2 else 1
        rows = slice(T * 128, (T + 1) * 128)
        SM = sm[i2]; PR = pr[i2]
        s2 = "sm2_%d" % i2; prk = "pr%d" % i2
        transposes(PR, prk, prT, "prT", 8)
        O = osb[i2]; ok = "osb%d" % i2
        for h in range(4):
            k = 4 + h % 2
            for c in range(2):
                b.op("pe", lambda e, h=h, c=c, k=k: e.matmul(
                    pf[k][:, :], lhsT=prT[:, 2 * h + c, :], rhs=vsb[:, a, c, h * 512:(h + 1) * 512],
                    start=(c == 0), stop=(c == 1)), r=["prT", "vsb"], w=[PF[k]])
            b.op("act", lambda e, h=h, k=k: e.activation(out=O[:, h * 512:(h + 1) * 512], in_=pf[k][:, :],
                                                         func=AF.Copy, scale=SM[:, 2, h:h + 1]),
                 r=[PF[k], s2], w=[ok])
        b.dma("pool", o_d[rows, :], O[:], r=[ok], w=[("o_d", T)])

    b_S0(0)
    for i in range(NT + 3):
        if i + 1 < NT:
            b_S0(i + 1)
        if i < NT:
            b_S1(i)
        if 2 <= i <= NT + 1:
            b_S3a(i - 2)
        if 1 <= i <= NT:
            b_S2(i - 1)
        if 2 <= i <= NT + 1:
            b_S3b(i - 2)
        if i >= 3:
            b_S4(i - 3)
    b.flush()
    st.close()

    st = contextlib.ExitStack()
    wo = load_w(st, "wo", w_o, D)
    gffn = SBt(st, "gffn", [128, D]); b.dma("sp", gffn[:], gffn_i, w=["gffn"])
    wr = SBt(st, "wr", [128, KD, NE]); b.dma("sp", wr[:], w_r.rearrange("(kc p) n -> p kc n", p=128), w=["wr"])
    br = SBt(st, "brt", [128, NE]); b.dma("sp", br[:], br_i, w=["br"])
    h1t = [SBt(st, "h1t%d" % i, [128, D]) for i in range(3)]
    ot = [SBt(st, "ot%d" % i, [128, D], BF16) for i in range(2)]
    oT = [SBt(st, "oT%d" % i, [128, KD, 128], BF16) for i in range(2)]
    h2 = [SBt(st, "h2_%d" % i, [128, D]) for i in range(2)]
    xn3 = [SBt(st, "xn3_%d" % i, [128, D]) for i in range(2)]
    xn3b = [SBt(st, "xn3b%d" % i, [128, D], BF16) for i in range(2)]
    xn3T = SBt(st, "xn3T", [128, KD, 128])
    lg = [SBt(st, "lg%d" % i, [128, NE]) for i in range(2)]
    sm = SBt(st, "sm", [128, 3])

    def c_S0(T):
        i2 = T % 2; i3 = T % 3
        rows = slice(T * 128, (T + 1) * 128)
        b.dma("sp", h1t[i3][:], h1_d[rows, :], w=["h1t%d" % i3])
        b.dma("sp", ot[i2][:], o_d[rows, :], w=["ot%d" % i2])

    def c_S1(T):
        i2 = T % 2
        transposes(ot[i2], "ot%d" % i2, oT[i2], "oT%d" % i2, KD)

    def c_S2(T):
        i2 = T % 2; i3 = T % 3
        rows = slice(T * 128, (T + 1) * 128)
        H = h1t[i3]; hk = "h1t%d" % i3
        H2 = h2[i2]; h2k = "h2_%d" % i2
        X3 = xn3[i2]; x3k = "xn3_%d" % i2

        def sink4(ch, ps, pk):
            b.op("dve", lambda e: e.tensor_tensor(out=H2[:, ch * 512:(ch + 1) * 512], in0=ps[:, :],
                                                  in1=H[:, ch * 512:(ch + 1) * 512], op=ALU.add),
                 r=[pk, hk], w=[h2k])
        proj(oT[i2], "oT%d" % i2, wo, "wo", D, sink4)
        if T < OWN:
            b.dma("pool", h2_d[rows, :], H2[:], r=[h2k], w=[("h2_d", T)])
        rs = rstd_of(H2[:], D, EPS, h2k, i2, "ssh%d" % i2, xn3b[i2][:], "xn3b%d" % i2)
        b.op("dve", lambda e: e.scalar_tensor_tensor(out=X3[:], in0=H2[:], scalar=rs, in1=gffn[:],
                                                     op0=ALU.mult, op1=ALU.mult),
             r=[h2k, "ssh%d" % i2, "gffn"], w=[x3k])
        if T < OWN:
            b.op("act", lambda e: e.copy(out=xn3b[i2][:], in_=X3[:]), r=[x3k], w=["xn3b%d" % i2])
            b.dma("pool", xn3_d[rows, :], xn3b[i2][:], r=["xn3b%d" % i2], w=[("xn3_d", T)])

    def c_S3(T):
        i2 = T % 2
        rows = slice(T * 128, (T + 1) * 128)
        transposes(xn3[i2], "xn3_%d" % i2, xn3T, "xn3T", KD, fp32=True, fbase=3)
        for kc in range(KD):
            b.op("pe", lambda e, kc=kc: e.matmul(pf[2][:, 0:NE], lhsT=xn3T[:, kc, :], rhs=wr[:, kc, :],
                                                 start=(kc == 0), stop=(kc == KD - 1)), r=["xn3T", "wr"], w=[PF[2]])
        Lg = lg[i2]; lk = "lg%d" % i2
        b.op("dve", lambda e: e.tensor_tensor(out=Lg[:], in0=pf[2][:, 0:NE], in1=br[:], op=ALU.add),
             r=[PF[2], "br"], w=[lk])
        b.op("dve", lambda e: e.tensor_reduce(out=sm[:, 0:1], in_=Lg[:], axis=AX.X, op=ALU.max), r=[lk], w=["sm0"])
        b.op("dve", lambda e: e.tensor_scalar(out=sm[:, 0:1], in0=sm[:, 0:1], scalar1=-1.0, scalar2=None, op0=ALU.mult),
             r=["sm0"], w=["sm0"])
        b.op("act", lambda e: e.activation(out=Lg[:], in_=Lg[:], func=AF.Exp, bias=sm[:, 0:1], scale=1.0,
                                           accum_out=sm[:, 1:2]), r=[lk, "sm0"], w=[lk, "sm1"])
        b.op("dve", lambda e: e.reciprocal(out=sm[:, 2:3], in_=sm[:, 1:2]), r=["sm1"], w=["sm2"])
        b.op("dve", lambda e: e.tensor_scalar(out=Lg[:], in0=Lg[:], scalar1=sm[:, 2:3], scalar2=None, op0=ALU.mult),
             r=[lk, "sm2"], w=[lk])
        b.dma("pool", aff_d[rows, :], Lg[:], r=[lk], w=[("aff_d", T)])

    c_S0(0)
    for i in range(NT + 2):
        if i + 1 < NT:
            c_S0(i + 1)
        if i < NT:
            c_S1(i)
        if 1 <= i <= NT:
            c_S2(i - 1)
        if i >= 2:
            c_S3(i - 2)
    b.flush()
    st.close()

    stB = contextlib.ExitStack()
    affg = SBt(stB, "affg", [128, NT, NE])
    b.dma("sp", affg[:], aff_d.rearrange("(t p) e -> p t e", p=128), w=["affg"])
    lo = SBt(stB, "lo", [128, NE]); hi = SBt(stB, "hi", [128, NE]); mid = SBt(stB, "mid", [128, NE])
    cmpt = SBt(stB, "cmpt", [128, NT, NE])
    cntp = SBt(stB, "cntp", [128, NE])
    onesf = SBt(stB, "onesf", [128, 128])
    b.dma("sp", onesf[:], ones_i, w=["onesf"])
    ge = SBt(stB, "ge", [128, NE]); tmp = SBt(stB, "tmp", [128, NE])
    sel = SBt(stB, "sel", [128, OWN, NE]); gate = SBt(stB, "gate", [128, OWN, NE]); slot = SBt(stB, "slot", [128, OWN, NE])
    selb = SBt(stB, "selb", [128, OWN, NE], BF16)
    off = SBt(stB, "off", [128, OWN, NE])
    b.op("dve", lambda e: e.memset(lo[:], 0.0), w=["lo"])
    b.op("dve", lambda e: e.memset(hi[:], 1.0), w=["hi"])
    b.op("dve", lambda e: e.memset(mid[:], 0.5), w=["mid"])
    for itn in range(34):
        b.op("dve", lambda e: e.tensor_tensor(out=cmpt[:], in0=affg[:], in1=mid[:, None, :].broadcast_to([128, NT, NE]),
                                              op=ALU.is_ge), r=["affg", "mid"], w=["cmpt"])
        b.op("dve", lambda e: e.tensor_reduce(out=cntp[:], in_=cmpt[:].rearrange("p t e -> p e t"), axis=AX.X, op=ALU.add),
             r=["cmpt"], w=["cntp"])
        b.op("pe", lambda e: e.matmul(pf[0][:, 0:NE], lhsT=onesf[:], rhs=cntp[:], start=True, stop=True),
             r=["onesf", "cntp"], w=[PF[0]])
        b.op("dve", lambda e: e.tensor_scalar(out=ge[:], in0=pf[0][:, 0:NE], scalar1=KSEL, scalar2=None, op0=ALU.is_ge),
             r=[PF[0]], w=["ge"])
        b.op("dve", lambda e: e.tensor_tensor(out=tmp[:], in0=ge[:], in1=mid[:], op=ALU.mult), r=["ge", "mid"], w=["tmp"])
        b.op("dve", lambda e: e.tensor_tensor(out=lo[:], in0=lo[:], in1=tmp[:], op=ALU.max), r=["lo", "tmp"], w=["lo"])
        b.op("dve", lambda e: e.scalar_tensor_tensor(out=tmp[:], in0=ge[:], scalar=4.0, in1=mid[:], op0=ALU.mult,
                                                     op1=ALU.add), r=["ge", "mid"], w=["tmp"])
        b.op("dve", lambda e: e.tensor_tensor(out=hi[:], in0=hi[:], in1=tmp[:], op=ALU.min), r=["hi", "tmp"], w=["hi"])
        b.op("dve", lambda e: e.tensor_tensor(out=mid[:], in0=lo[:], in1=hi[:], op=ALU.add), r=["lo", "hi"], w=["mid"])
        b.op("dve", lambda e: e.tensor_scalar(out=mid[:], in0=mid[:], scalar1=0.5, scalar2=None, op0=ALU.mult),
             r=["mid"], w=["mid"])
    if debug:
        b.dma("sp", dbg["d_thr"], lo[:], r=["lo"])
    b.op("dve", lambda e: e.tensor_tensor(out=sel[:], in0=affg[:, 0:OWN, :], in1=lo[:, None, :].broadcast_to([128, OWN, NE]),
                                          op=ALU.is_ge), r=["affg", "lo"], w=["sel"])
    b.op("dve", lambda e: e.tensor_tensor(out=gate[:], in0=affg[:, 0:OWN, :], in1=sel[:], op=ALU.mult),
         r=["affg", "sel"], w=["gate"])
    b.op("dve", lambda e: e.tensor_copy(out=selb[:], in_=sel[:]), r=["sel"], w=["selb"])
    sflat = selb[:].rearrange("p t e -> p (t e)")
    b.op("pe", lambda e: e.matmul(pf[1][:, 0:OWN * NE], lhsT=tri[:], rhs=sflat, start=True, stop=True),
         r=["tri", "selb"], w=[PF[1]])
    b.op("pe", lambda e: e.matmul(pf[2][:, 0:OWN * NE], lhsT=ones[:], rhs=sflat, start=True, stop=True),
         r=["ones", "selb"], w=[PF[2]])
    b.op("dve", lambda e: e.memset(off[:], 0.0), w=["off"])
    for T in range(1, OWN):
        b.op("dve", lambda e, T=T: e.tensor_tensor(out=off[:, T, :], in0=off[:, T - 1, :],
                                                   in1=pf[2][:, (T - 1) * NE:T * NE], op=ALU.add),
             r=["off", PF[2]], w=["off"])
    b.op("dve", lambda e: e.tensor_tensor(out=slot[:].rearrange("p t e -> p (t e)"), in0=pf[1][:, 0:OWN * NE],
                                          in1=off[:].rearrange("p t e -> p (t e)"), op=ALU.add),
         r=[PF[1], "off"], w=["slot"])
    st = contextlib.ExitStack()
    xn3s = SBt(st, "xn3s", [128, OWN, D], BF16)
    for T in range(OWN):
        b.dma("sp", xn3s[:, T, :], xn3_d[T * 128:(T + 1) * 128, :], w=["xn3s"])
    b.flush()

    Pt = [SBt(st, "Pt%d" % i, [128, CAP], BF16) for i in range(4)]
    xsT = SBt(st, "xsT", [128, KD, CAP], BF16)
    hT = SBt(st, "hT", [128, FC, CAP], BF16)
    FBW = 256
    wg = [SBt(st, "wg%d" % i, [128, KD, FBW], BF16) for i in range(2)]
    wu = [SBt(st, "wu%d" % i, [128, KD, FBW], BF16) for i in range(2)]
    WDB = 4
    wd = [SBt(st, "wd%d" % i, [128, WDB, 512], BF16) for i in range(2)]
    ysb = SBt(st, "ysb", [128, NCC, D], BF16)
    sg = [SBt(st, "sg%d" % i, [128, CAP]) for i in range(2)]
    wblk = 0
    dblk = 0
    for ex in range(NE):
        for dg in range(KD // 4):
            for T in range(OWN):
                P = Pt[T % 4]; pk = "Pt%d" % (T % 4)
                b.op("dve", lambda e, P=P, T=T, ex=ex: e.tensor_scalar(
                    out=P[:], in0=iota[:], scalar1=slot[:, T, ex:ex + 1], scalar2=sel[:, T, ex:ex + 1],
                    op0=ALU.is_equal, op1=ALU.mult), r=["iota", "slot", "sel"], w=[pk])
                for dl in range(4):
                    dc = dg * 4 + dl
                    b.op("pe", lambda e, P=P, T=T, dc=dc, dl=dl: e.matmul(
                        pf[dl][:, 0:CAP], lhsT=xn3s[:, T, dc * 128:(dc + 1) * 128], rhs=P[:],
                        start=(T == 0), stop=(T == OWN - 1)), r=["xn3s", pk], w=[PF[dl]])
            for dl in range(4):
                evac(xsT[:, dg * 4 + dl, :], pf[dl][:, 0:CAP], r=[PF[dl]], w=["xsT"])
        wgv = w_gate[ex].rearrange("(kc p) f -> p kc f", p=128)
        wuv = w_up[ex].rearrange("(kc p) f -> p kc f", p=128)
        for fb in range(DE // FBW):
            i2 = wblk % 2; wblk += 1
            b.dma("pool", wg[i2][:], wgv[:, :, fb * FBW:(fb + 1) * FBW], w=["wg%d" % i2])
            b.dma("pool", wu[i2][:], wuv[:, :, fb * FBW:(fb + 1) * FBW], w=["wu%d" % i2])
            for fl in range(FBW // 128):
                fc = fb * (FBW // 128) + fl
                kg, ku = (4, 5) if fc % 2 == 0 else (2, 3)
                for kc in range(KD):
                    b.op("pe", lambda e, kc=kc, fl=fl, i2=i2, kg=kg: e.matmul(
                        pf[kg][:, 0:CAP], lhsT=wg[i2][:, kc, fl * 128:(fl + 1) * 128], rhs=xsT[:, kc, :],
                        start=(kc == 0), stop=(kc == KD - 1)), r=["wg%d" % i2, "xsT"], w=[PF[kg]])
                for kc in range(KD):
                    b.op("pe", lambda e, kc=kc, fl=fl, i2=i2, ku=ku: e.matmul(
                        pf[ku][:, 0:CAP], lhsT=wu[i2][:, kc, fl * 128:(fl + 1) * 128], rhs=xsT[:, kc, :],
                        start=(kc == 0), stop=(kc == KD - 1)), r=["wu%d" % i2, "xsT"], w=[PF[ku]])
                S = sg[fc % 2]; sk = "sg%d" % (fc % 2)
                b.op("act", lambda e, S=S, kg=kg: e.activation(out=S[:], in_=pf[kg][:, 0:CAP], func=AF.Silu), r=[PF[kg]], w=[sk])
                b.op("dve", lambda e, S=S, fc=fc, ku=ku: e.tensor_tensor(out=hT[:, fc, :], in0=pf[ku][:, 0:CAP], in1=S[:],
                                                                         op=ALU.mult), r=[PF[ku], sk], w=["hT"])
        wdv = w_down[ex].rearrange("(fc p) d -> p fc d", p=128)
        for dch in range(4):
            for fcb in range(FC // WDB):
                i2 = dblk % 2; dblk += 1
                b.dma("pool", wd[i2][:], wdv[:, fcb * WDB:(fcb + 1) * WDB, dch * 512:(dch + 1) * 512], w=["wd%d" % i2])
                for fl in range(WDB):
                    fc = fcb * WDB + fl
                    for ncc in range(NCC):
                        kb = (dch % 2) * 3 + ncc
                        b.op("pe", lambda e, fc=fc, fl=fl, ncc=ncc, i2=i2, kb=kb: e.matmul(
                            pf[kb][:, :], lhsT=hT[:, fc, ncc * 128:(ncc + 1) * 128], rhs=wd[i2][:, fl, :],
                            start=(fc == 0), stop=(fc == FC - 1)), r=["hT", "wd%d" % i2], w=[PF[kb]])
            for ncc in range(NCC):
                kb = (dch % 2) * 3 + ncc
                evac(ysb[:, ncc, dch * 512:(dch + 1) * 512], pf[kb][:, :], r=[PF[kb]], w=["ysb"])
        b.dma("sp", yex_d[ex].rearrange("(c p) d -> p c d", p=128), ysb[:], r=["ysb"], w=[("yex_d", ex)])
    b.flush()
    st.close()

    st = contextlib.ExitStack()
    gfin = SBt(st, "gfin", [128, D]); b.dma("sp", gfin[:], gfin_i, w=["gfin"])
    Pg = [SBt(st, "Pg%d" % i, [128, CAP], BF16) for i in range(2)]
    PTg = [SBt(st, "PTg%d" % i, [128, NE, NCC, 128], BF16) for i in range(4)]
    yl = [SBt(st, "yl%d" % i, [128, NCC, D // 2], BF16) for i in range(3)]
    h2t = [SBt(st, "h2t%d" % i, [128, D]) for i in range(4)]
    h3 = [SBt(st, "h3_%d" % i, [128, D]) for i in range(2)]
    yo = [SBt(st, "yo%d" % i, [128, D]) for i in range(2)]
    yblk = 0
    pcount = 0
    for r in range(OWN // 2):
        tiles = [2 * r, 2 * r + 1]
        PTs = []
        for tl, T in enumerate(tiles):
            hb = (2 * r + tl) % 4
            b.dma("sp", h2t[hb][:], h2_d[T * 128:(T + 1) * 128, :], w=["h2t%d" % hb])
            PT = PTg[hb]; ptk = "PTg%d" % hb
            PTs.append((PT, ptk, hb))
            for ex in range(NE):
                P = Pg[pcount % 2]; pk = "Pg%d" % (pcount % 2)
                bank = pcount % 2
                pcount += 1
                b.op("dve", lambda e, P=P, T=T, ex=ex: e.tensor_scalar(
                    out=P[:], in0=iota[:], scalar1=slot[:, T, ex:ex + 1], scalar2=gate[:, T, ex:ex + 1],
                    op0=ALU.is_equal, op1=ALU.mult), r=["iota", "slot", "gate"], w=[pk])
                for c in range(NCC):
                    b.op("pe", lambda e, P=P, c=c, bank=bank: e.transpose(ptr[bank][:, c * 128:(c + 1) * 128],
                                                                         P[:, c * 128:(c + 1) * 128], ident[:]),
                         r=[pk, "ident"], w=[PTR[bank]])
                evac(PT[:, ex, :, :], ptr[bank][:, 0:NCC * 128].rearrange("p (a c) -> p a c", a=NCC),
                     r=[PTR[bank]], w=[ptk])
        for dh in range(2):
            for ex in range(NE):
                j3 = yblk % 3; yblk += 1
                b.dma("sp", yl[j3][:], yex_d[ex].rearrange("(c p) d -> p c d", p=128)[:, :, dh * (D // 2):(dh + 1) * (D // 2)],
                      w=["yl%d" % j3])
                for tl in range(2):
                    PT, ptk, hb = PTs[tl]
                    for dc in range(2):
                        k = tl * 2 + dc
                        for c in range(NCC):
                            b.op("pe", lambda e, ex=ex, c=c, dc=dc, j3=j3, PT=PT, k=k: e.matmul(
                                pf[k][:, :], lhsT=PT[:, ex, c, :], rhs=yl[j3][:, c, dc * 512:(dc + 1) * 512],
                                start=(ex == 0 and c == 0), stop=(ex == NE - 1 and c == NCC - 1)),
                                r=[ptk, "yl%d" % j3], w=[PF[k]])
            for tl in range(2):
                PT, ptk, hb = PTs[tl]
                for dc in range(2):
                    k = tl * 2 + dc
                    col = dh * (D // 2) + dc * 512
                    b.op("dve", lambda e, k=k, col=col, tl=tl, hb=hb: e.tensor_tensor(
                        out=h3[tl][:, col:col + 512], in0=pf[k][:, :], in1=h2t[hb][:, col:col + 512],
                        op=ALU.add), r=[PF[k], "h2t%d" % hb], w=["h3_%d" % tl])
        for tl, T in enumerate(tiles):
            rs = rstd_of(h3[tl][:], D, EPS, "h3_%d" % tl, tl, "ss%d" % tl, yo[tl][:], "yo%d" % tl)
            b.op("dve", lambda e, tl=tl, rs=rs: e.scalar_tensor_tensor(out=yo[tl][:], in0=h3[tl][:], scalar=rs, in1=gfin[:],
                                                                      op0=ALU.mult, op1=ALU.mult),
                 r=["h3_%d" % tl, "ss%d" % tl, "gfin"], w=["yo%d" % tl])
            b.dma("pool", y_out[T * 128:(T + 1) * 128, :], yo[tl][:], r=["yo%d" % tl], w=[("y", T)])
    b.flush(last=True)
    st.close()
    stB.close()
    b.close()
    top.close()
    return nc


def _tile_perm(x, order):
    return np.ascontiguousarray(x.reshape(-1, 128, x.shape[-1])[order].reshape(-1, x.shape[-1]))


def _dft_tables(order, NT, two_seq, bseq):
    JH = NT // 2
    order = np.asarray(order)
    q = np.arange(128)
    f = np.arange(128)
    if not two_seq:
        NTs = NT
        S = NT * 128
        tloc = order
        seq_of = np.zeros(NT, dtype=int)
        half_seq = [0, 0]
    else:
        NTs = NT // 2
        S = NTs * 128
        tloc = order % NTs
        seq_of = order // NTs
        half_seq = [bseq, 1 - bseq]
    c1 = np.zeros((NT, 2, 3, 128), dtype=np.float64)
    for m in range(2):
        ang = -2 * np.pi * np.outer(tloc, q) / NTs
        msk = (seq_of == half_seq[m]).astype(np.float64)[:, None]
        re = np.cos(ang) * msk
        im = np.sin(ang) * msk
        c1[:, m, 0, :] = re
        c1[:, m, 1, :] = im
        c1[:, m, 2, :] = -re
    m2re = np.zeros((128, 2, 128, JH), dtype=np.float64)
    m2im = np.zeros((128, 2, 128, JH), dtype=np.float64)
    for m in range(2):
        jj = tloc[m * JH:(m + 1) * JH]
        ang = -2 * np.pi * (f[:, None, None] * jj[None, None, :] / NTs + f[:, None, None] * q[None, :, None] / S)
        m2re[:, m] = np.cos(ang)
        m2im[:, m] = -np.sin(ang)
    c = np.arange(128)
    nrm = 1.0 / math.sqrt(S * 128.0)
    ang = 2 * np.pi * np.outer(c, c) / 128
    cc = np.cos(ang) * nrm
    sc = np.sin(ang) * nrm
    f32 = lambda a: np.ascontiguousarray(a.astype(np.float32))
    return dict(c1=f32(c1), m2re=f32(m2re.reshape(128, -1)), m2im=f32(m2im.reshape(128, -1)), cc=f32(cc), sc=f32(sc))


def _core_inputs(inp, NT, OWN, CAP, cfgs):
    rep = lambda vv: np.ascontiguousarray(np.broadcast_to(np.asarray(vv, np.float32).reshape(1, -1), (128, np.asarray(vv).size)))
    NE = inp["w_router"].shape[-1]
    shared = dict(
        w_in=np.ascontiguousarray(inp["w_in"][0]), w_spT=np.ascontiguousarray(np.transpose(inp["w_spatial"][0], (0, 2, 1))),
        bsp=np.ascontiguousarray(inp["b_spatial"][0].T), lng=rep(inp["ln_v_g"][0]), lnb=rep(inp["ln_v_b"][0]),
        w_fourier=np.ascontiguousarray(inp["w_fourier"][0]), goa=rep(inp["norm_out_a"][0]), gob=rep(inp["norm_out_b"][0]),
        w_out=np.ascontiguousarray(inp["w_out"][0]), gmix=rep(inp["norm_mix"][0]), gattn=rep(inp["norm_attn"][0]),
        gmem=rep(inp["norm_mem"][0]), gffn=rep(inp["norm_ffn"][0]), gfin=rep(inp["norm_final"]),
        w_q=np.ascontiguousarray(inp["w_q"][0]), w_kv=np.ascontiguousarray(inp["w_kv"][0]),
        w_o=np.ascontiguousarray(inp["w_o"][0]), w_router=np.ascontiguousarray(inp["w_router"][0]),
        br=rep(inp["b_router"][0]), w_gate=np.ascontiguousarray(inp["w_gate"][0]), w_up=np.ascontiguousarray(inp["w_up"][0]),
        w_down=np.ascontiguousarray(inp["w_down"][0]),
        ident=np.eye(128, dtype=np.float32), ones=np.ones((128, 128), np.float32),
        tri=np.triu(np.ones((128, 128), np.float32), 1),
        iota=np.ascontiguousarray(np.broadcast_to(np.arange(CAP, dtype=np.float32)[None, :], (128, CAP))),
    )
    maps = []
    for (xgrp, order, two_seq, bseq, mA, mB) in cfgs:
        m = dict(shared)
        m["xg"] = _tile_perm(xgrp, order)
        m["memA"] = np.ascontiguousarray(mA)
        m["memB"] = np.ascontiguousarray(mB)
        m.update(_dft_tables(order, NT, two_seq, bseq))
        maps.append(m)
    return maps


_NC_CACHE = {}


def kernel(**inp):
    inp = {k: np.asarray(v) for k, v in inp.items()}
    NT, OWN, CAP = 64, 16, 384
    NE = inp["w_router"].shape[-1]
    xp = inp["x_prompt"][0]
    xs = inp["x_sample"].reshape(-1, D)
    cfgs = []
    orders = []
    for c in range(4):
        own = list(range(16 * c, 16 * c + 16))
        order = own + [t for t in range(NT) if t not in own]
        orders.append(order)
        cfgs.append((xp, order, False, 0, inp["mem_prompt"][0], inp["mem_prompt"][0]))
    for cs in range(4):
        bq = cs // 2
        own = list(range(32 * bq + 16 * (cs % 2), 32 * bq + 16 * (cs % 2) + 16))
        rest = [t for t in range(32 * bq, 32 * bq + 32) if t not in own]
        other = list(range(32 * (1 - bq), 32 * (1 - bq) + 32))
        order = own + rest + other
        orders.append(order)
        cfgs.append((xs, order, True, bq, inp["mem_sample"][bq], inp["mem_sample"][1 - bq]))
    key = (NT, OWN, NE, CAP)
    if key not in _NC_CACHE:
        _NC_CACHE[key] = build(NT, OWN, NE, CAP)
    nc = _NC_CACHE[key]
    maps = _core_inputs(inp, NT, OWN, CAP, cfgs)
    res = run_bass_kernel_spmd(nc, maps, core_ids=list(range(8)))
    yp = np.zeros((1, 8192, D), np.float32)
    ys = np.zeros((2, 4096, D), np.float32)
    ysf = ys.reshape(-1, D)
    for c in range(8):
        y = np.asarray(res.results[c]["y"], dtype=np.float32)
        own = orders[c][:OWN]
        dst = yp[0] if c < 4 else ysf
        for i, t in enumerate(own):
            dst[t * 128:(t + 1) * 128] = y[i * 128:(i + 1) * 128]
    return (yp, ys)
```
